# Optimizing a Trainium2 kernel written in Bass

```python
import jax, jax.numpy as jnp
from jax import lax
import numpy as np

D_MODEL = 2048
BATCH = 2
SEQ = 16384
DEPTH = 2

GRID_W = 64
CTX_LEN = 256
HEAD_DIM = 128
N_Q_HEADS = 16
N_KV_HEADS = 4
GROUP = N_Q_HEADS // N_KV_HEADS
WINDOW = 128
BLOCK = 128
ROPE_BASE = 10000.0
ROT_FREQS = HEAD_DIM // 4
CONV_WIDTH = D_MODEL
CONV_K = 3
N_EXPERTS = 32
N_EXPERT_GROUPS = 4
EXPERTS_PER_GROUP = N_EXPERTS // N_EXPERT_GROUPS
TOP_K = 2
EXPERT_FF = 1024
MOE_BLOCK = 256
Q_WIDTH = N_Q_HEADS * HEAD_DIM
KV_WIDTH = N_KV_HEADS * HEAD_DIM
IN_WIDTH = Q_WIDTH + 2 * KV_WIDTH + 3 * CONV_WIDTH + 2 * D_MODEL
ALPHA = (2 * DEPTH) ** 0.25
BETA = (8 * DEPTH) ** -0.25
LN_EPS = 1e-5
NEG_INF = -1e30
ATTN_SCALE = HEAD_DIM ** -0.5

kernel_name = "hybrid_swa_shortconv_groupedmoe_deepnorm_dit"


def layer_norm(x, g, b):
    xf = x.astype(jnp.float32)
    mu = xf.mean(-1, keepdims=True)
    var = jnp.square(xf - mu).mean(-1, keepdims=True)
    return ((xf - mu) * lax.rsqrt(var + LN_EPS) * g + b).astype(x.dtype)


def axial_rope_tables(n):
    t = jnp.arange(n, dtype=jnp.int32)
    row = (t // GRID_W).astype(jnp.float32)
    col = (t % GRID_W).astype(jnp.float32)
    inv_freq = jnp.power(ROPE_BASE, -jnp.arange(ROT_FREQS, dtype=jnp.float32) / ROT_FREQS)
    ang = jnp.stack([row[:, None] * inv_freq, col[:, None] * inv_freq], axis=1)
    return jnp.cos(ang), jnp.sin(ang)


def apply_rope(x, cos, sin):
    b, n, h, d = x.shape
    xr = x.astype(jnp.float32).reshape(b, n, h, 2, 2, ROT_FREQS)
    x1, x2 = xr[..., 0, :], xr[..., 1, :]
    cs, sn = cos[None, :, None], sin[None, :, None]
    out = jnp.stack([x1 * cs - x2 * sn, x2 * cs + x1 * sn], axis=-2)
    return out.reshape(x.shape).astype(x.dtype)


def split_in_proj(p):
    base = Q_WIDTH + 2 * KV_WIDTH
    cuts = [Q_WIDTH, Q_WIDTH + KV_WIDTH, base, base + CONV_WIDTH, base + 2 * CONV_WIDTH,
            base + 3 * CONV_WIDTH, base + 3 * CONV_WIDTH + D_MODEL]
    return jnp.split(p, cuts, axis=-1)


def latent_window_attention(q, k, v, kc, vc, sink):
    b, n = q.shape[0], q.shape[1]
    n_blocks = n // BLOCK
    pad = ((0, 0), (BLOCK, BLOCK), (0, 0), (0, 0))
    kp, vp = jnp.pad(k, pad), jnp.pad(v, pad)
    sink_l = sink.astype(jnp.float32).reshape(N_KV_HEADS, GROUP)
    band = jnp.arange(3 * BLOCK)
    qi = jnp.arange(BLOCK)

    def block(blk):
        q0 = blk * BLOCK
        qb = lax.dynamic_slice_in_dim(q, q0, BLOCK, axis=1).reshape(b, BLOCK, N_KV_HEADS, GROUP, HEAD_DIM)
        kb = lax.dynamic_slice_in_dim(kp, q0, 3 * BLOCK, axis=1)
        vb = lax.dynamic_slice_in_dim(vp, q0, 3 * BLOCK, axis=1)
        qpos = q0 + qi
        kpos = q0 - BLOCK + band
        mask = (jnp.abs(kpos[None, :] - qpos[:, None]) <= WINDOW) & (kpos >= 0)[None, :] & (kpos < n)[None, :]
        s_loc = jnp.einsum('bqhgd,bshd->bhgqs', qb, kb).astype(jnp.float32) * ATTN_SCALE
        s_loc = jnp.where(mask, s_loc, NEG_INF)
        s_ctx = jnp.einsum('bqhgd,bchd->bhgqc', qb, kc).astype(jnp.float32) * ATTN_SCALE
        s_sink = jnp.broadcast_to(sink_l[None, :, :, None, None], (b, N_KV_HEADS, GROUP, BLOCK, 1))
        p = jax.nn.softmax(jnp.concatenate([s_loc, s_ctx, s_sink], axis=-1), axis=-1)
        p_loc = p[..., :3 * BLOCK].astype(v.dtype)
        p_ctx = p[..., 3 * BLOCK:3 * BLOCK + kc.shape[1]].astype(v.dtype)
        o = jnp.einsum('bhgqs,bshd->bqhgd', p_loc, vb) + jnp.einsum('bhgqc,bchd->bqhgd', p_ctx, vc)
        return o.reshape(b, BLOCK, Q_WIDTH)

    o = lax.map(block, jnp.arange(n_blocks))
    return o.transpose(1, 0, 2, 3).reshape(b, n, Q_WIDTH)


def context_attention(qc, kc, vc, sink):
    b, cl = qc.shape[0], qc.shape[1]
    qb = qc.reshape(b, cl, N_KV_HEADS, GROUP, HEAD_DIM)
    s = jnp.einsum('bqhgd,bshd->bhgqs', qb, kc).astype(jnp.float32) * ATTN_SCALE
    s_sink = jnp.broadcast_to(sink.astype(jnp.float32).reshape(N_KV_HEADS, GROUP)[None, :, :, None, None],
                              (b, N_KV_HEADS, GROUP, cl, 1))
    p = jax.nn.softmax(jnp.concatenate([s, s_sink], axis=-1), axis=-1)[..., :cl].astype(vc.dtype)
    return jnp.einsum('bhgqs,bshd->bqhgd', p, vc).reshape(b, cl, Q_WIDTH)


def short_conv(u, w):
    half = CONV_K // 2
    n = u.shape[1]
    up = jnp.pad(u, ((0, 0), (half, half), (0, 0)))
    return sum(up[:, j:j + n] * w[j] for j in range(CONV_K))


def merge_branches(attn_o, b_gate, c_gate, u, g_attn, g_conv, conv_w, w_attn_proj, w_conv_proj, w_out):
    a = attn_o @ w_attn_proj
    s = (b_gate * short_conv(c_gate * u, conv_w)) @ w_conv_proj
    m = jax.nn.sigmoid(g_attn) * a + jax.nn.sigmoid(g_conv) * s
    return m @ w_out


def grouped_moe(h, router_w, router_b, w_gate, w_up, w_down):
    t, d = h.shape
    probs = jax.nn.softmax((h @ router_w).astype(jnp.float32), axis=-1)
    sel = probs + router_b.astype(jnp.float32)
    sel_g = sel.reshape(t, N_EXPERT_GROUPS, EXPERTS_PER_GROUP)
    group_score = lax.top_k(sel_g, TOP_K)[0].sum(-1)
    g_best = jnp.argmax(group_score, axis=-1)
    in_group = jnp.take_along_axis(sel_g, g_best[:, None, None], axis=1)[:, 0]
    local_idx = lax.top_k(in_group, TOP_K)[1]
    expert_idx = g_best[:, None] * EXPERTS_PER_GROUP + local_idx
    wts = jnp.take_along_axis(probs, expert_idx, axis=-1)
    wts = wts / wts.sum(-1, keepdims=True)

    n_assign = t * TOP_K
    e_flat = expert_idx.reshape(-1).astype(jnp.int32)
    tok_flat = jnp.repeat(jnp.arange(t, dtype=jnp.int32), TOP_K)
    w_flat = wts.reshape(-1)
    order = jnp.argsort(e_flat)
    e_s, tok_s, w_s = e_flat[order], tok_flat[order], w_flat[order]
    counts = jnp.zeros((N_EXPERTS,), jnp.int32).at[e_flat].add(1)
    padded = (counts + MOE_BLOCK - 1) // MOE_BLOCK * MOE_BLOCK
    pend = jnp.cumsum(padded)
    pstart = pend - padded
    start = jnp.cumsum(counts) - counts
    dest = pstart[e_s] + jnp.arange(n_assign, dtype=jnp.int32) - start[e_s]
    cap = (n_assign + N_EXPERTS * (MOE_BLOCK - 1) + MOE_BLOCK - 1) // MOE_BLOCK * MOE_BLOCK
    n_blk = cap // MOE_BLOCK
    slot_tok = jnp.full((cap,), t, jnp.int32).at[dest].set(tok_s)
    slot_w = jnp.zeros((cap,), jnp.float32).at[dest].set(w_s)
    blk_expert = jnp.minimum(jnp.searchsorted(pend, jnp.arange(n_blk, dtype=jnp.int32) * MOE_BLOCK, side='right'),
                             N_EXPERTS - 1)
    h_pad = jnp.concatenate([h, jnp.zeros((1, d), h.dtype)], axis=0)
    xs = h_pad[slot_tok].reshape(n_blk, MOE_BLOCK, d)

    def expert_block(args):
        xb, e = args
        return (jax.nn.silu(xb @ w_gate[e]) * (xb @ w_up[e])) @ w_down[e]

    ys = lax.map(expert_block, (xs, blk_expert)).reshape(cap, d)
    out = jnp.zeros((t + 1, d), ys.dtype).at[slot_tok].add(ys * slot_w[:, None].astype(ys.dtype))
    return out[:t]


def setup_inputs(seed: int = 0) -> dict:
    key = jax.random.key(seed)
    ks = jax.random.split(key, 24)

    def nrm(k, shape, scale):
        return jax.random.normal(k, shape, jnp.float32) * scale

    return {
        "x": nrm(ks[0], (BATCH, SEQ, D_MODEL), 1.0),
        "c": nrm(ks[1], (BATCH, D_MODEL), 1.0),
        "ctx": nrm(ks[2], (BATCH, CTX_LEN, D_MODEL), 1.0),
        "c_ctx": nrm(ks[3], (D_MODEL,), 1.0),
        "w_ada": nrm(ks[4], (DEPTH, D_MODEL, 6 * D_MODEL), 0.5 * D_MODEL ** -0.5),
        "b_ada": nrm(ks[5], (DEPTH, 6 * D_MODEL), 0.01),
        "w_in": nrm(ks[6], (DEPTH, D_MODEL, IN_WIDTH), D_MODEL ** -0.5),
        "attn_sink": nrm(ks[7], (DEPTH, N_Q_HEADS), 0.5),
        "conv_w": nrm(ks[8], (DEPTH, CONV_K, CONV_WIDTH), CONV_K ** -0.5),
        "w_attn_proj": nrm(ks[9], (DEPTH, Q_WIDTH, D_MODEL), Q_WIDTH ** -0.5),
        "w_conv_proj": nrm(ks[10], (DEPTH, CONV_WIDTH, D_MODEL), CONV_WIDTH ** -0.5),
        "w_out": nrm(ks[11], (DEPTH, D_MODEL, D_MODEL), BETA * D_MODEL ** -0.5),
        "ln1_g": 1.0 + nrm(ks[12], (DEPTH, D_MODEL), 0.02),
        "ln1_b": nrm(ks[13], (DEPTH, D_MODEL), 0.02),
        "ln2_g": 1.0 + nrm(ks[14], (DEPTH, D_MODEL), 0.02),
        "ln2_b": nrm(ks[15], (DEPTH, D_MODEL), 0.02),
        "router_w": nrm(ks[16], (D_MODEL, N_EXPERTS), D_MODEL ** -0.5),
        "router_b": nrm(ks[17], (N_EXPERTS,), 0.01),
        "w_gate": nrm(ks[18], (DEPTH, N_EXPERTS, D_MODEL, EXPERT_FF), D_MODEL ** -0.5),
        "w_up": nrm(ks[19], (DEPTH, N_EXPERTS, D_MODEL, EXPERT_FF), D_MODEL ** -0.5),
        "w_down": nrm(ks[20], (DEPTH, N_EXPERTS, EXPERT_FF, D_MODEL), BETA * EXPERT_FF ** -0.5),
    }


def reference(x, c, ctx, c_ctx, w_ada, b_ada, w_in, attn_sink, conv_w, w_attn_proj, w_conv_proj, w_out,
              ln1_g, ln1_b, ln2_g, ln2_b, router_w, router_b, w_gate, w_up, w_down):
    b, n, d = x.shape
    cl = ctx.shape[1]
    cos, sin = axial_rope_tables(n)
    silu_c = jax.nn.silu(c)
    silu_cc = jax.nn.silu(c_ctx)
    xc = ctx
    for l in range(DEPTH):
        last = l == DEPTH - 1
        mod = (silu_c @ w_ada[l] + b_ada[l]).reshape(b, 6, d)[:, :, None, :]
        modc = (silu_cc @ w_ada[l] + b_ada[l]).reshape(6, d)

        h = x * (1 + mod[:, 1]) + mod[:, 0]
        hc = xc * (1 + modc[1]) + modc[0]
        q, k, v, cb, cc, cu, ga, gc = split_in_proj(h @ w_in[l])
        q = apply_rope(q.reshape(b, n, N_Q_HEADS, HEAD_DIM), cos, sin)
        k = apply_rope(k.reshape(b, n, N_KV_HEADS, HEAD_DIM), cos, sin)
        v = v.reshape(b, n, N_KV_HEADS, HEAD_DIM)
        if last:
            kc, vc = jnp.split(hc @ w_in[l][:, Q_WIDTH:Q_WIDTH + 2 * KV_WIDTH], 2, axis=-1)
        else:
            qc, kc, vc, cbc, ccc, cuc, gac, gcc = split_in_proj(hc @ w_in[l])
        kc = kc.reshape(b, cl, N_KV_HEADS, HEAD_DIM)
        vc = vc.reshape(b, cl, N_KV_HEADS, HEAD_DIM)

        attn = latent_window_attention(q, k, v, kc, vc, attn_sink[l])
        y = merge_branches(attn, cb, cc, cu, ga, gc, conv_w[l], w_attn_proj[l], w_conv_proj[l], w_out[l])
        x = layer_norm(ALPHA * x + mod[:, 2] * y, ln1_g[l], ln1_b[l])
        if not last:
            attn_c = context_attention(qc, kc, vc, attn_sink[l])
            yc = merge_branches(attn_c, cbc, ccc, cuc, gac, gcc, conv_w[l], w_attn_proj[l], w_conv_proj[l], w_out[l])
            xc = layer_norm(ALPHA * xc + modc[2] * yc, ln1_g[l], ln1_b[l])

        h2 = (x * (1 + mod[:, 4]) + mod[:, 3]).reshape(b * n, d)
        if last:
            f = grouped_moe(h2, router_w, router_b, w_gate[l], w_up[l], w_down[l]).reshape(b, n, d)
            x = layer_norm(ALPHA * x + mod[:, 5] * f, ln2_g[l], ln2_b[l])
        else:
            h2c = (xc * (1 + modc[4]) + modc[3]).reshape(b * cl, d)
            f = grouped_moe(jnp.concatenate([h2, h2c], axis=0), router_w, router_b, w_gate[l], w_up[l], w_down[l])
            x = layer_norm(ALPHA * x + mod[:, 5] * f[:b * n].reshape(b, n, d), ln2_g[l], ln2_b[l])
            xc = layer_norm(ALPHA * xc + modc[5] * f[b * n:].reshape(b, cl, d), ln2_g[l], ln2_b[l])
    return x
```

```python
import numpy as np
from contextlib import ExitStack
import concourse.bass as bass
import concourse.mybir as mybir
from concourse.bass_utils import run_bass_kernel_spmd

F32 = mybir.dt.float32
BF16 = mybir.dt.bfloat16
AF = mybir.ActivationFunctionType
ALU = mybir.AluOpType
AX = mybir.AxisListType

D = 2048
KC = 16
TL = 4608
TC = 256
TT = TL + TC
OWN = 4096
HALO = 256
INW = 13312
NE = 32
ALPHA = 4.0 ** 0.25
LN_EPS = 1e-5
SCALE = 128.0 ** -0.5
PT = 1792


class Buf:
    __slots__ = ("w", "r", "excl")

    def __init__(self, excl=False):
        self.w = None
        self.r = {}
        self.excl = excl


class Sched:
    ENG = ("pe", "act", "dve", "pool", "sp")

    def __init__(self, nc, stack, ndma=(("sp", 32), ("pool", 32))):
        self.nc = nc
        self.prog = {e: [] for e in self.ENG}
        self.esem = {}
        self.ecnt = {e: 0 for e in self.ENG}
        self.seen = {e: {} for e in self.ENG}
        for e in self.ENG:
            self.esem[e] = stack.enter_context(nc.semaphore(f"es_{e}"))
        self.dring = {}
        self.dpos = {}
        for q, n in ndma:
            self.dring[q] = [[f"d_{q}{i}", stack.enter_context(nc.semaphore(f"d_{q}{i}")), 0] for i in range(n)]
            self.dpos[q] = 0

    def _wait(self, e, tok):
        if tok is None:
            return
        key, sem, val = tok
        if self.seen[e].get(key, 0) >= val:
            return
        self.seen[e][key] = val
        self.prog[e].append(("wait", sem, val))

    def _deps(self, e, reads, writes, pe_chain=False):
        for b in reads:
            self._wait(e, b.w)
        for b in writes:
            if not (pe_chain and b.w is not None and b.w[0] == "es_pe"):
                self._wait(e, b.w)
            for k, (s, v) in b.r.items():
                self._wait(e, (k, s, v))

    def _mark(self, tok, reads, writes):
        key, sem, val = tok
        for b in reads:
            b.r[key] = (sem, val)
        for b in writes:
            b.w = tok
            b.r = {}

    def op(self, e, fn, reads=(), writes=(), pe_chain=False):
        writes = list(writes) + [b for b in reads if b.excl]
        reads = [b for b in reads if not b.excl]
        self._deps(e, reads, writes, pe_chain)
        self.ecnt[e] += 1
        tok = (f"es_{e}", self.esem[e], self.ecnt[e])
        self.prog[e].append(("op", fn, self.esem[e], 1))
        self._mark(tok, reads, writes)
        return tok

    def dma(self, q, fn, reads=(), writes=()):
        ring = self.dring[q]
        slot = ring[self.dpos[q] % len(ring)]
        self.dpos[q] += 1
        key, sem, val = slot
        if val > 0:
            self._wait(q, (key, sem, val))
        self._deps(q, reads, writes)
        slot[2] = val + 16
        tok = (key, sem, val + 16)
        self.prog[q].append(("op", fn, sem, 16))
        self._mark(tok, reads, writes)
        return tok

    def barrier(self):
        toks = []
        for e in self.ENG:
            if self.ecnt[e] > 0:
                toks.append((f"es_{e}", self.esem[e], self.ecnt[e]))
        for q, ring in self.dring.items():
            for key, sem, val in ring:
                if val > 0:
                    toks.append((key, sem, val))
        for e in self.ENG:
            for t in toks:
                self._wait(e, t)

    def emit(self):
        nc = self.nc
        engmap = {"pe": "tensor", "act": "scalar", "dve": "vector", "pool": "gpsimd", "sp": "sync"}
        with nc.Block() as block:
            for e in self.ENG:
                prog = self.prog[e]

                def body(eng, prog=prog):
                    for item in prog:
                        if item[0] == "wait":
                            eng.wait_ge(item[1], item[2])
                        else:
                            item[1](eng).then_inc(item[2], item[3])
                getattr(block, engmap[e])(body)
        self.prog = {e: [] for e in self.ENG}


class Ring:
    def __init__(self, items):
        self.items = items
        self.i = 0

    def next(self):
        it = self.items[self.i % len(self.items)]
        self.i += 1
        return it


def layer_groups(l):
    if l == 0:
        lat_in = [(t, 512, False) for t in range(0, TL, 512)]
        lat = [(t, 512, False) for t in range(128, 4224, 512)] + [(4224, 256, False)]
        ctxg = [(TL, 256, True)]
        return lat_in + ctxg, lat + ctxg
    lat_in = [(t, 512, False) for t in range(128, 4224, 512)] + [(4224, 256, False)]
    lat = [(t, 512, False) for t in range(256, 4352, 512)]
    return lat_in + [(TL, 256, True)], lat


def build(nlayers=2, upto=99, taps=(), small=(), dbg=(), moe="sorted"):
    nc = bass.Bass("TRN2", target_bir_lowering=False)

    def din(name, shape, dt=F32):
        if name in small:
            shape = [1] * len(shape)
        return nc.dram_tensor(name, list(shape), dt, kind="ExternalInput").ap()

    def dscr(name, shape, dt):
        kind = "ExternalOutput" if name in taps else "Internal"
        return nc.dram_tensor(name, list(shape), dt, kind=kind).ap()

    xin = din("xin", [KC, 128, TT])
    cvec = din("cvec", [128, 32])
    badaT = din("badaT", [128, 2 * 96])
    lnp = din("lnp", [128, 2 * 4 * 16])
    cwt_d = din("cwt", [128, 2 * 48])
    sinkrow = din("sinkrow", [2, 2048])
    rw_d = din("rw", [128, 16 * 32])
    rb_d = din("rb", [128, 32])
    cos_d = din("cosT", [128, TL])
    sin_d = din("sinT", [128, TL])
    vbias_d = din("vbias", [128, 36])
    vrow_d = din("vrow", [128, TL])
    consts_d = din("consts", [128, 128 * 2 + 512 * 2 + 32 * 128])
    thr_d = din("thr", [128, 129])
    w_ada = din("w_ada", [2, D, 6 * D])
    w_in = din("w_in", [2, D, INW])
    w_ap = din("w_attn_proj", [2, D, D])
    w_cp = din("w_conv_proj", [2, D, D])
    w_o = din("w_out", [2, D, D])
    NROWS = 2 * NE * 2 * 128
    w_g = din("w_gate", [NROWS * 4, 2048])
    w_u = din("w_up", [NROWS * 4, 2048])
    w_d = din("w_down", [NROWS * 4, 2048])
    yout = nc.dram_tensor("yout", [KC, 128, OWN], F32, kind="ExternalOutput").ap()

    qT = dscr("qT", [16, 128, TT], BF16)
    kT = dscr("kT", [4, 128, TT], BF16)
    vtok = dscr("vtok", [TT, 512], BF16)
    plT = dscr("plT", [80, 128, TT], BF16)
    maT = dscr("maT", [16, 128, TT], BF16)
    mT = dscr("mT", [16, 128, TT], BF16)
    x1T = dscr("x1T", [16, 128, TT], F32)
    h2T = dscr("h2T", [16, 128, TT], BF16)
    fT = dscr("fT", [16, 128, TT], F32)
    x2T = dscr("x2T", [16, 128, TT], F32)
    cwTd = dscr("cwTd", [32, TT], BF16)
    modd = dscr("modd", [2, 128, 192], F32)
    I32 = mybir.dt.int32
    NBMAX = 104
    h2tok = dscr("h2tok", [TT, D], BF16)
    xsd = dscr("xsd", [NBMAX * 128, D], BF16)
    ysd = dscr("ysd", [NBMAX * 128, D], F32)
    slotd = dscr("slotd", [128, 76], I32)
    blked = dscr("blked", [128, 256], I32)
    attd = dscr("attd", [16, 128, TT], BF16)

    with ExitStack() as st:
        S = Sched(nc, st)

        uid = [0]

        def sb(stack, name, shape, dt):
            uid[0] += 1
            return stack.enter_context(nc.sbuf_tensor(f"{name}_u{uid[0]}", list(shape), dt))

        psb = [st.enter_context(nc.psum_tensor(f"ps{i}", [128, 512], F32)) for i in range(8)]
        psB = [Buf(True) for _ in range(8)]
        identb = sb(st, "identb", [128, 128], BF16)
        pswapb = sb(st, "pswapb", [128, 128], BF16)
        triLb = sb(st, "triLb", [128, 512], BF16)
        triUb = sb(st, "triUb", [128, 512], BF16)
        selEb = sb(st, "selEb", [32, 32 * 128], BF16)
        onesb = sb(st, "onesb", [128, 128], BF16)
        onesf = sb(st, "onesf", [128, 128], F32)
        cv = sb(st, "cv", [128, 32], F32)
        scT = sb(st, "scT", [128, 32], F32)
        bada = sb(st, "bada", [128, 192], F32)
        lnt = sb(st, "lnt", [128, 128], F32)
        cwt = sb(st, "cwt_sb", [128, 96], F32)
        rwt = sb(st, "rwt", [128, 512], F32)
        rbt = sb(st, "rbt", [128, 32], F32)
        vbias = sb(st, "vbias_sb", [128, 36], F32)
        modT = [sb(st, f"modT{l}", [128, 192], F32) for l in range(2)]
        modP = [sb(st, f"modP{l}", [128, 192], F32) for l in range(2)]
        cwT = sb(st, "cwT", [32, TT if moe == "dense" else 2], BF16)
        OH = sb(st, "OH", [128, 38 * 64], F32)
        WW = sb(st, "WW", [128, 76], F32)
        POS = sb(st, "POS", [128, 38 * 32], F32)
        SLOT = sb(st, "SLOT", [128, 76], I32)
        BLKE = sb(st, "BLKE", [128, 128], I32)
        CHG = sb(st, "CHG", [128, 128], I32)
        thr = sb(st, "thr_sb", [128, 129], F32)
        IDXT = sb(st, "IDXT", [128, 1024], I32)
        ustr = sb(st, "ustr", [128, 128], BF16)
        brout = Buf()
        bslot = Buf()
        bblk = Buf()
        psbf = [psb[6][:, :].bitcast(BF16), psb[7][:, :].bitcast(BF16)]
        tmpst = ExitStack()
        consts_f = sb(tmpst, "consts_f", [128, 128 * 2 + 512 * 2], F32)
        selEf = sb(tmpst, "selEf", [32, 32 * 128], F32)
        bconst = Buf()
        bmod = [Buf(), Buf()]
        bcwT = Buf()

        CO_ID, CO_PS, CO_TL, CO_TU, CO_SE = 0, 128, 256, 768, 1280
        S.dma("sp", lambda e: e.dma_start(out=consts_f[:, :], in_=consts_d[:, 0:1280]), writes=[bconst])
        S.dma("sp", lambda e: e.dma_start(out=selEf[:, :], in_=consts_d[0:32, 1280:1280 + 4096]), writes=[bconst])
        for (dst, src) in ((cv, cvec), (bada, badaT), (lnt, lnp), (cwt, cwt_d), (rwt, rw_d), (rbt, rb_d), (vbias, vbias_d), (thr, thr_d)):
            S.dma("sp", lambda e, dst=dst, src=src: e.dma_start(out=dst[:, :], in_=src[:, :]), writes=[bconst])
        S.op("dve", lambda e: e.tensor_copy(out=identb[:, :], in_=consts_f[:, CO_ID:CO_ID + 128]), reads=[bconst], writes=[bconst])
        S.op("dve", lambda e: e.tensor_copy(out=pswapb[:, :], in_=consts_f[:, CO_PS:CO_PS + 128]), reads=[bconst], writes=[bconst])
        S.op("dve", lambda e: e.tensor_copy(out=triLb[:, :], in_=consts_f[:, CO_TL:CO_TL + 512]), reads=[bconst], writes=[bconst])
        S.op("dve", lambda e: e.tensor_copy(out=triUb[:, :], in_=consts_f[:, CO_TU:CO_TU + 512]), reads=[bconst], writes=[bconst])
        S.op("dve", lambda e: e.tensor_copy(out=selEb[:, :], in_=selEf[:, :]), reads=[bconst], writes=[bconst])
        S.op("dve", lambda e: e.tensor_tensor(out=ustr[:, :], in0=consts_f[:, CO_TU:CO_TU + 128], in1=consts_f[:, CO_ID:CO_ID + 128], op=ALU.subtract), reads=[bconst], writes=[bconst])
        S.op("pool", lambda e: e.memset(onesb[:, :], 1.0), writes=[bconst])
        S.op("pool", lambda e: e.memset(onesf[:, :], 1.0), writes=[bconst])
        S.op("act", lambda e: e.activation(out=scT[:, :], in_=cv[:, :], func=AF.Silu), reads=[bconst], writes=[bconst])
        S.barrier()

        def phase_mod(l):
            with ExitStack() as ph:
                wst = Ring([(sb(ph, f"wada{i}", [128, 16 * 512], F32), Buf()) for i in range(2)])
                for jb in range(24):
                    wt, bw = wst.next()
                    S.dma("sp", lambda e, wt=wt, jb=jb: e.dma_start(
                        out=wt[:, :].rearrange("p (k c) -> p k c", k=16),
                        in_=w_ada[l, :, jb * 512:(jb + 1) * 512].rearrange("(k p) c -> p k c", p=128)), writes=[bw])
                    for cc in range(4):
                        j = jb * 4 + cc
                        pi = j % 2
                        for k in range(16):
                            S.op("pe", lambda e, wt=wt, k=k, cc=cc, pi=pi: e.matmul(
                                psb[pi][:, 0:2], wt[:, k * 512 + cc * 128:k * 512 + (cc + 1) * 128], scT[:, 2 * k:2 * k + 2],
                                start=(k == 0), stop=(k == 15)), reads=[bw, bconst], writes=[psB[pi]], pe_chain=(k > 0))
                        S.op("dve", lambda e, j=j, pi=pi: e.tensor_scalar(
                            out=modT[l][:, 2 * j:2 * j + 2], in0=psb[pi][:, 0:2], scalar1=bada[:, l * 96 + j:l * 96 + j + 1], scalar2=None,
                            op0=ALU.add), reads=[psB[pi], bconst], writes=[bmod[l]])
                S.op("dve", lambda e: e.tensor_scalar(out=modP[l][:, :], in0=modT[l][:, :], scalar1=1.0, scalar2=None, op0=ALU.add),
                     reads=[bmod[l]], writes=[bmod[l]])
                if "modd" in taps:
                    S.dma("sp", lambda e: e.dma_start(out=modd[l], in_=modT[l][:, :]), reads=[bmod[l]])
                S.barrier()
                S.emit()

        def mcol(l, i, k, s, plus=False):
            t = modP[l] if plus else modT[l]
            c = 2 * (i * 16 + k) + s
            return t[:, c:c + 1]

        def phase_inproj(l, xres):
            groups_in, _ = layer_groups(l)
            passes = []
            cur = []
            tot = 0
            for g in groups_in:
                if tot + g[1] > PT:
                    passes.append(cur)
                    cur, tot = [], 0
                cur.append(g)
                tot += g[1]
            passes.append(cur)
            with ExitStack() as ph:
                hT = sb(ph, "hT", [128, 16 * PT], BF16)
                bh = Buf()
                xst = Ring([(sb(ph, f"xst{i}", [128, 16 * 512], F32), Buf()) for i in range(1)])
                wbr = Ring([(sb(ph, f"wb{i}", [128, 16 * 256], BF16), Buf()) for i in range(3)])
                cost = sb(ph, "cost", [128, PT], F32)
                sint = sb(ph, "sint", [128, PT], F32)
                btab = Buf()
                qbr = Ring([(sb(ph, f"qb{i}", [128, 512], BF16), Buf()) for i in range(2)])
                t1r = Ring([(sb(ph, f"t1_{i}", [128, 512], F32), Buf()) for i in range(2)])
                t2r = Ring([(sb(ph, f"t2_{i}", [128, 512], F32), Buf()) for i in range(2)])
                obr = Ring([(sb(ph, f"ob{i}", [128, 512], BF16), Buf()) for i in range(4)])
                psr = Ring([(psb[i], psB[i]) for i in range(4)])
                ps2r = Ring([(psb[i], psB[i]) for i in range(4, 6)])
                evi = [0]
                for pgroups in passes:
                    offs = []
                    off = 0
                    for (t0, n, isc) in pgroups:
                        offs.append(off)
                        off += n
                    for (t0, n, isc), off in zip(pgroups, offs):
                        xt, bx = xst.next()
                        S.dma("sp", lambda e, xt=xt, t0=t0, n=n: e.dma_start(
                            out=xt[:, 0:16 * n].rearrange("p (k t) -> p k t", k=16),
                            in_=xres[:, :, t0:t0 + n].rearrange("k p t -> p k t")), writes=[bx])
                        s = 1 if isc else 0
                        for k in range(16):
                            S.op("act", lambda e, xt=xt, k=k, n=n, off=off, s=s: e.activation(
                                out=hT[:, k * PT + off:k * PT + off + n], in_=xt[:, k * n:(k + 1) * n], func=AF.Identity,
                                bias=mcol(l, 0, k, s), scale=mcol(l, 1, k, s, True)), reads=[bx, bmod[l]], writes=[bh])
                        if not isc:
                            S.dma("sp", lambda e, t0=t0, n=n, off=off: e.dma_start(out=cost[:, off:off + n], in_=cos_d[:, t0:t0 + n]), writes=[btab])
                            S.dma("sp", lambda e, t0=t0, n=n, off=off: e.dma_start(out=sint[:, off:off + n], in_=sin_d[:, t0:t0 + n]), writes=[btab])
                    for cb in range(INW // 256):
                        c0 = cb * 256
                        isv = 2560 <= c0 < 3072
                        iskv = 2048 <= c0 < 3072
                        if l == 1 and all(g[2] for g in pgroups) and not iskv:
                            continue
                        if 'cb4' in dbg and cb >= 4 and not (cb in (10, 11) and 'v' in dbg):
                            continue
                        wb, bw = wbr.next()
                        S.dma("pool", lambda e, wb=wb, c0=c0: e.dma_start(
                            out=wb[:, :].rearrange("p (k c) -> p k c", k=16),
                            in_=w_in[l, :, c0:c0 + 256].rearrange("(k p) c -> p k c", p=128)), writes=[bw])
                        if isv and 'nov' in dbg:
                            continue
                        if isv:
                            for (t0, n, isc), off in zip(pgroups, offs):
                                for ti in range(n // 128):
                                    ps, bp = psr.next()
                                    for k in range(16):
                                        S.op("pe", lambda e, ps=ps, wb=wb, k=k, o=off + ti * 128: e.matmul(
                                            ps[:, 0:256], hT[:, k * PT + o:k * PT + o + 128], wb[:, k * 256:(k + 1) * 256],
                                            start=(k == 0), stop=(k == 15)), reads=[bw, bh], writes=[bp], pe_chain=(k > 0))
                                    ob, bo = obr.next()
                                    S.op("act", lambda e, ob=ob, ps=ps: e.activation(out=ob[:, 0:256], in_=ps[:, 0:256], func=AF.Copy),
                                         reads=[bp], writes=[bo])
                                    r0 = t0 + ti * 128
                                    S.dma("sp", lambda e, ob=ob, r0=r0, c0=c0: e.dma_start(
                                        out=vtok[r0:r0 + 128, c0 - 2560:c0 - 2560 + 256], in_=ob[:, 0:256]), reads=[bo])
                            continue
                        for cc in range(2):
                            gc = cb * 2 + cc
                            for (t0, n, isc), off in zip(pgroups, offs):
                                if l == 1 and isc and not iskv:
                                    continue
                                ps, bp = psr.next()
                                for k in range(16):
                                    S.op("pe", lambda e, ps=ps, wb=wb, k=k, cc=cc, off=off, n=n: e.matmul(
                                        ps[:, 0:n], wb[:, k * 256 + cc * 128:k * 256 + (cc + 1) * 128], hT[:, k * PT + off:k * PT + off + n],
                                        start=(k == 0), stop=(k == 15)), reads=[bw, bh], writes=[bp], pe_chain=(k > 0))
                                if gc < 16:
                                    dst = qT[gc]
                                elif gc < 20:
                                    dst = kT[gc - 16]
                                else:
                                    dst = plT[gc - 24]
                                ob, bo = obr.next()
                                if gc < 20 and not isc and 'norope' not in dbg:
                                    qb, bq = qbr.next()
                                    S.op("act", lambda e, qb=qb, ps=ps, n=n: e.activation(out=qb[:, 0:n], in_=ps[:, 0:n], func=AF.Copy),
                                         reads=[bp], writes=[bq])
                                    ps2, bp2 = ps2r.next()
                                    S.op("pe", lambda e, ps2=ps2, qb=qb, n=n: e.matmul(ps2[:, 0:n], pswapb[:, :], qb[:, 0:n], start=True, stop=True),
                                         reads=[bq, bconst], writes=[bp2])
                                    t1, b1 = t1r.next()
                                    t2, b2 = t2r.next()
                                    S.op("dve", lambda e, t1=t1, ps=ps, n=n, off=off: e.tensor_tensor(
                                        out=t1[:, 0:n], in0=ps[:, 0:n], in1=cost[:, off:off + n], op=ALU.mult), reads=[bp, btab], writes=[b1])
                                    S.op("dve", lambda e, t2=t2, ps2=ps2, n=n, off=off: e.tensor_tensor(
                                        out=t2[:, 0:n], in0=ps2[:, 0:n], in1=sint[:, off:off + n], op=ALU.mult), reads=[bp2, btab], writes=[b2])
                                    S.op("dve" if "ropedve" in dbg else "pool", lambda e, ob=ob, t1=t1, t2=t2, n=n: e.tensor_tensor(
                                        out=ob[:, 0:n], in0=t1[:, 0:n], in1=t2[:, 0:n], op=ALU.add), reads=[b1, b2], writes=[bo])
                                else:
                                    evi[0] += 1
                                    if evi[0] % 2 == 0:
                                        S.op("act", lambda e, ob=ob, ps=ps, n=n: e.activation(out=ob[:, 0:n], in_=ps[:, 0:n], func=AF.Copy),
                                             reads=[bp], writes=[bo])
                                    else:
                                        S.op("dve", lambda e, ob=ob, ps=ps, n=n: e.tensor_copy(out=ob[:, 0:n], in_=ps[:, 0:n]),
                                             reads=[bp], writes=[bo])
                                S.dma("sp", lambda e, ob=ob, dst=dst, t0=t0, n=n: e.dma_start(out=dst[:, t0:t0 + n], in_=ob[:, 0:n]), reads=[bo])
                S.barrier()
                S.emit()

        def load_w2048(ph, name, wd, l):
            wt = sb(ph, name, [128, 16 * 2048], BF16)
            bw = Buf()
            for c in range(4):
                S.dma("pool", lambda e, c=c: e.dma_start(
                    out=wt[:, :].rearrange("p (k c) -> p k c", k=16)[:, :, c * 512:(c + 1) * 512],
                    in_=wd[l, :, c * 512:(c + 1) * 512].rearrange("(k p) c -> p k c", p=128)), writes=[bw])
            return wt, bw

        def phase_attn(l):
            _, groups = layer_groups(l)
            with ExitStack() as ph:
                wa, bwa = load_w2048(ph, "wa", w_ap, l)
                kcT = sb(ph, "kcT", [128, 4 * 256], BF16)
                vcs = sb(ph, "vcs", [128, 2 * 512], BF16)
                bkc = Buf()
                S.dma("sp", lambda e: e.dma_start(out=kcT[:, :].rearrange("p (h t) -> p h t", h=4),
                                                  in_=kT[:, :, TL:TL + 256].rearrange("h p t -> p h t")), writes=[bkc])
                S.dma("sp", lambda e: e.dma_start(out=vcs[:, :].rearrange("p (j c) -> p j c", j=2),
                                                  in_=vtok[TL:TL + 256, :].rearrange("(j p) c -> p j c", p=128)), writes=[bkc])
                sk_f = sb(ph, "sk_f", [1, 2048], F32)
                esink = sb(ph, "esink", [1, 2048], BF16)
                bsk = Buf()
                S.dma("sp", lambda e: e.dma_start(out=sk_f[:, :], in_=sinkrow[l:l + 1, :]), writes=[bsk])
                S.op("act", lambda e: e.activation(out=esink[:, :], in_=sk_f[:, :], func=AF.Exp), reads=[bsk], writes=[bsk])
                qsr = Ring([(sb(ph, f"qs{i}", [128, 16 * 512], BF16), Buf()) for i in range(1)])
                ksr = Ring([(sb(ph, f"ks{i}", [128, 4 * 768], BF16), Buf()) for i in range(2)])
                vsr = Ring([(sb(ph, f"vs{i}", [128, 6 * 512], BF16), Buf()) for i in range(2)])
                atr = Ring([(sb(ph, f"at{i}", [128, 16 * 512], BF16), Buf()) for i in range(1)])
                ptr = Ring([(sb(ph, f"pt{i}", [128, 512], BF16), Buf()) for i in range(4)])
                rdr = Ring([(sb(ph, f"rd{i}", [128, 512], F32), Buf()) for i in range(2)])
                gar = Ring([(sb(ph, f"ga{i}", [128, 512], BF16), Buf()) for i in range(2)])
                sgr = Ring([(sb(ph, f"sg{i}", [128, 512], F32), Buf()) for i in range(2)])
                mar = Ring([(sb(ph, f"mao{i}", [128, 512], BF16), Buf()) for i in range(2)])
                pss = Ring([(psb[i], psB[i]) for i in (0, 1)])
                pso = Ring([(psb[i], psB[i]) for i in (2, 3)])
                psd = Ring([(psb[i], psB[i]) for i in (4, 5)])
                psa = Ring([(psb[i], psB[i]) for i in (6, 7)])
                for (t0, n, isc) in groups:
                    nt = n // 128
                    qs, bqs = qsr.next()
                    S.dma("sp", lambda e, qs=qs, t0=t0, n=n: e.dma_start(
                        out=qs[:, 0:16 * n].rearrange("p (h t) -> p h t", h=16), in_=qT[:, :, t0:t0 + n].rearrange("h p t -> p h t")), writes=[bqs])
                    if not isc:
                        kw = n + 256
                        ks, bks = ksr.next()
                        vs, bvs = vsr.next()
                        S.dma("sp", lambda e, ks=ks, t0=t0, kw=kw: e.dma_start(
                            out=ks[:, 0:4 * kw].rearrange("p (h t) -> p h t", h=4),
                            in_=kT[:, :, t0 - 128:t0 - 128 + kw].rearrange("h p t -> p h t")), writes=[bks])
                        S.dma("sp", lambda e, vs=vs, t0=t0, kw=kw: e.dma_start(
                            out=vs[:, 0:(kw // 128) * 512].rearrange("p (j c) -> p j c", c=512),
                            in_=vtok[t0 - 128:t0 - 128 + kw, :].rearrange("(j p) c -> p j c", p=128)), writes=[bvs])
                    at, bat = atr.next()
                    for i in range(nt):
                        for h in range(4):
                            kts = []
                            if not isc:
                                for kk in range(3):
                                    gt = t0 // 128 - 1 + i + kk
                                    kts.append((ks[:, h * kw + (i + kk) * 128:h * kw + (i + kk + 1) * 128],
                                                vs[:, (i + kk) * 512 + h * 128:(i + kk) * 512 + (h + 1) * 128],
                                                vbias[:, gt:gt + 1], (triLb if kk == 0 else (triUb if kk == 2 else None)), [bks, bvs]))
                            for j in range(2):
                                kts.append((kcT[:, h * 256 + j * 128:h * 256 + (j + 1) * 128],
                                            vcs[:, j * 512 + h * 128:j * 512 + (h + 1) * 128], 0.0, None, [bkc]))
                            po, bpo = pso.next()
                            pd, bpd = psd.next()
                            qv = qs[:, 0:16 * n].rearrange("p (h t) -> p h t", h=16)[:, 4 * h:4 * h + 4, i * 128:(i + 1) * 128]
                            nk = len(kts)
                            pts = [None] * nk
                            for ki in range(nk + 1):
                                if ki < nk:
                                    kap, vap, bias, msk, deps = kts[ki]
                                    p_s, bps_ = pss.next()
                                    S.op("pe", lambda e, p_s=p_s, kap=kap, qv=qv: e.matmul(
                                        p_s[:, 0:512].rearrange("p (g q) -> p g q", g=4), kap, qv, start=True, stop=True),
                                        reads=deps + [bqs], writes=[bps_])
                                    pt, bpt = ptr.next()
                                    pts[ki] = (pt, bpt)
                                    S.op("act", lambda e, pt=pt, p_s=p_s, bias=bias: e.activation(
                                        out=pt[:, :], in_=p_s[:, 0:512], func=AF.Exp, bias=bias, scale=SCALE), reads=[bps_, bconst], writes=[bpt])
                                    if msk is not None:
                                        S.op("pool", lambda e, pt=pt, msk=msk: e.tensor_tensor(out=pt[:, :], in0=pt[:, :], in1=msk[:, :], op=ALU.mult),
                                             reads=[bpt, bconst], writes=[bpt])
                                if ki >= 1:
                                    kj = ki - 1
                                    kap, vap, bias, msk, deps = kts[kj]
                                    pt, bpt = pts[kj]
                                    S.op("pe", lambda e, po=po, vap=vap, pt=pt, kj=kj, nk=nk: e.matmul(po[:, 0:512], vap, pt[:, :], start=(kj == 0), stop=(kj == nk - 1)),
                                         reads=deps + [bpt], writes=[bpo], pe_chain=(kj > 0))
                                    S.op("pe", lambda e, pd=pd, pt=pt, kj=kj: e.matmul(pd[:, 0:512], onesb[:, :], pt[:, :], start=(kj == 0), stop=False),
                                         reads=[bpt, bconst], writes=[bpd], pe_chain=(kj > 0))
                            S.op("pe", lambda e, pd=pd, h=h: e.matmul(pd[:, 0:512], onesb[0:1, :], esink[0:1, h * 512:(h + 1) * 512], start=False, stop=True),
                                 reads=[bsk, bconst], writes=[bpd], pe_chain=True)
                            rd, brd = rdr.next()
                            S.op("dve", lambda e, rd=rd, pd=pd: e.reciprocal(out=rd[:, :], in_=pd[:, 0:512]), reads=[bpd], writes=[brd])
                            av = at[:, 0:16 * n].rearrange("p (h t) -> p h t", h=16)[:, 4 * h:4 * h + 4, i * 128:(i + 1) * 128]
                            S.op("dve", lambda e, av=av, po=po, rd=rd: e.tensor_tensor(
                                out=av, in0=po[:, 0:512].rearrange("p (g q) -> p g q", g=4), in1=rd[:, :].rearrange("p (g q) -> p g q", g=4), op=ALU.mult),
                                reads=[bpo, brd], writes=[bat])
                    if "attd" in taps:
                        S.dma("sp", lambda e, at=at, t0=t0, n=n: e.dma_start(out=attd[:, :, t0:t0 + n].rearrange("h p t -> p h t"),
                                                                            in_=at[:, 0:16 * n].rearrange("p (h t) -> p h t", h=16)), reads=[bat])
                    for c in range(16):
                        pa, bpa = psa.next()
                        for k in range(16):
                            S.op("pe", lambda e, pa=pa, k=k, c=c, at=at, n=n: e.matmul(
                                pa[:, 0:n], wa[:, k * 2048 + c * 128:k * 2048 + (c + 1) * 128], at[:, k * n:(k + 1) * n],
                                start=(k == 0), stop=(k == 15)), reads=[bwa, bat], writes=[bpa], pe_chain=(k > 0))
                        ga, bga = gar.next()
                        S.dma("sp", lambda e, ga=ga, c=c, t0=t0, n=n: e.dma_start(out=ga[:, 0:n], in_=plT[48 + c][:, t0:t0 + n]), writes=[bga])
                        sg, bsg = sgr.next()
                        S.op("act", lambda e, sg=sg, ga=ga, n=n: e.activation(out=sg[:, 0:n], in_=ga[:, 0:n], func=AF.Sigmoid), reads=[bga], writes=[bsg])
                        mo, bmo = mar.next()
                        S.op("dve", lambda e, mo=mo, pa=pa, sg=sg, n=n: e.tensor_tensor(out=mo[:, 0:n], in0=pa[:, 0:n], in1=sg[:, 0:n], op=ALU.mult),
                             reads=[bpa, bsg], writes=[bmo])
                        S.dma("sp", lambda e, mo=mo, c=c, t0=t0, n=n: e.dma_start(out=maT[c][:, t0:t0 + n], in_=mo[:, 0:n]), reads=[bmo])
                S.barrier()
                S.emit()

        def phase_conv(l):
            _, groups = layer_groups(l)
            with ExitStack() as ph:
                ws, bws = load_w2048(ph, "ws", w_cp, l)
                vrow = sb(ph, "vrow", [128, TL], F32)
                bvr = Buf()
                S.dma("sp", lambda e: e.dma_start(out=vrow[:, :], in_=vrow_d[:, :]), writes=[bvr])
                zr = Ring([(sb(ph, f"z{i}", [128, 16 * 512], BF16), Buf()) for i in range(2)])
                cbr = Ring([(sb(ph, f"cb{i}", [128, 512], BF16), Buf()) for i in range(2)])
                ccr = Ring([(sb(ph, f"cc{i}", [128, 514], BF16), Buf()) for i in range(2)])
                cur = Ring([(sb(ph, f"cu{i}", [128, 514], BF16), Buf()) for i in range(2)])
                ur = Ring([(sb(ph, f"u{i}", [128, 514], F32), Buf()) for i in range(2)])
                tr = Ring([(sb(ph, f"t{i}", [128, 512], F32), Buf()) for i in range(2)])
                gcr = Ring([(sb(ph, f"gc{i}", [128, 512], BF16), Buf()) for i in range(2)])
                mair = Ring([(sb(ph, f"mai{i}", [128, 512], BF16), Buf()) for i in range(2)])
                sgr = Ring([(sb(ph, f"sg{i}", [128, 512], F32), Buf()) for i in range(2)])
                t3r = Ring([(sb(ph, f"t3_{i}", [128, 512], F32), Buf()) for i in range(2)])
                mor = Ring([(sb(ph, f"mo{i}", [128, 512], BF16), Buf()) for i in range(2)])
                psr = Ring([(psb[i], psB[i]) for i in range(4)])
                for (t0, n, isc) in groups:
                    z, bz = zr.next()
                    for k in range(16):
                        cbt, bcb = cbr.next()
                        cct, bcc = ccr.next()
                        cut, bcu = cur.next()
                        S.dma("sp", lambda e, cbt=cbt, k=k, t0=t0, n=n: e.dma_start(out=cbt[:, 0:n], in_=plT[k][:, t0:t0 + n]), writes=[bcb])
                        if isc:
                            S.op("pool", lambda e, cct=cct, n=n: e.memset(cct[:, 0:n + 2], 0.0), writes=[bcc])
                            S.dma("sp", lambda e, cct=cct, k=k, t0=t0, n=n: e.dma_start(out=cct[:, 1:n + 1], in_=plT[16 + k][:, t0:t0 + n]), writes=[bcc])
                            S.dma("sp", lambda e, cut=cut, k=k, t0=t0, n=n: e.dma_start(out=cut[:, 1:n + 1], in_=plT[32 + k][:, t0:t0 + n]), writes=[bcu])
                            S.op("pool", lambda e, cut=cut, n=n: e.memset(cut[:, 0:1], 0.0), reads=[bcu], writes=[bcu])
                            S.op("pool", lambda e, cut=cut, n=n: e.memset(cut[:, n + 1:n + 2], 0.0), reads=[bcu], writes=[bcu])
                        else:
                            S.dma("sp", lambda e, cct=cct, k=k, t0=t0, n=n: e.dma_start(out=cct[:, 0:n + 2], in_=plT[16 + k][:, t0 - 1:t0 + n + 1]), writes=[bcc])
                            S.dma("sp", lambda e, cut=cut, k=k, t0=t0, n=n: e.dma_start(out=cut[:, 0:n + 2], in_=plT[32 + k][:, t0 - 1:t0 + n + 1]), writes=[bcu])
                        u, bu = ur.next()
                        S.op("pool", lambda e, u=u, cct=cct, cut=cut, n=n: e.tensor_tensor(out=u[:, 0:n + 2], in0=cct[:, 0:n + 2], in1=cut[:, 0:n + 2], op=ALU.mult),
                             reads=[bcc, bcu], writes=[bu])
                        if not isc:
                            S.op("pool", lambda e, u=u, t0=t0, n=n: e.tensor_tensor(out=u[:, 0:n + 2], in0=u[:, 0:n + 2], in1=vrow[:, t0 - 1:t0 + n + 1], op=ALU.mult),
                                 reads=[bu, bvr], writes=[bu])
                        t, bt = tr.next()
                        wc = lambda j, k=k: cwt[:, l * 48 + k * 3 + j:l * 48 + k * 3 + j + 1]
                        S.op("dve", lambda e, t=t, u=u, n=n, wc=wc: e.tensor_scalar(out=t[:, 0:n], in0=u[:, 0:n], scalar1=wc(0), scalar2=None, op0=ALU.mult),
                             reads=[bu, bconst], writes=[bt])
                        S.op("dve", lambda e, t=t, u=u, n=n, wc=wc: e.scalar_tensor_tensor(out=t[:, 0:n], in0=u[:, 1:n + 1], scalar=wc(1), in1=t[:, 0:n], op0=ALU.mult, op1=ALU.add),
                             reads=[bu, bconst, bt], writes=[bt])
                        S.op("dve", lambda e, t=t, u=u, n=n, wc=wc: e.scalar_tensor_tensor(out=t[:, 0:n], in0=u[:, 2:n + 2], scalar=wc(2), in1=t[:, 0:n], op0=ALU.mult, op1=ALU.add),
                             reads=[bu, bconst, bt], writes=[bt])
                        S.op("pool", lambda e, z=z, k=k, n=n, cbt=cbt, t=t: e.tensor_tensor(out=z[:, k * n:(k + 1) * n], in0=cbt[:, 0:n], in1=t[:, 0:n], op=ALU.mult),
                             reads=[bcb, bt], writes=[bz])
                    for c in range(16):
                        ps, bp = psr.next()
                        for k in range(16):
                            S.op("pe", lambda e, ps=ps, k=k, c=c, z=z, n=n: e.matmul(
                                ps[:, 0:n], ws[:, k * 2048 + c * 128:k * 2048 + (c + 1) * 128], z[:, k * n:(k + 1) * n],
                                start=(k == 0), stop=(k == 15)), reads=[bws, bz], writes=[bp], pe_chain=(k > 0))
                        gct, bgc = gcr.next()
                        mai, bmai = mair.next()
                        S.dma("sp", lambda e, gct=gct, c=c, t0=t0, n=n: e.dma_start(out=gct[:, 0:n], in_=plT[64 + c][:, t0:t0 + n]), writes=[bgc])
                        S.dma("sp", lambda e, mai=mai, c=c, t0=t0, n=n: e.dma_start(out=mai[:, 0:n], in_=maT[c][:, t0:t0 + n]), writes=[bmai])
                        sg, bsg = sgr.next()
                        S.op("act", lambda e, sg=sg, gct=gct, n=n: e.activation(out=sg[:, 0:n], in_=gct[:, 0:n], func=AF.Sigmoid), reads=[bgc], writes=[bsg])
                        t3, bt3 = t3r.next()
                        S.op("dve", lambda e, t3=t3, ps=ps, sg=sg, n=n: e.tensor_tensor(out=t3[:, 0:n], in0=ps[:, 0:n], in1=sg[:, 0:n], op=ALU.mult),
                             reads=[bp, bsg], writes=[bt3])
                        mo, bmo = mor.next()
                        S.op("pool", lambda e, mo=mo, t3=t3, mai=mai, n=n: e.tensor_tensor(out=mo[:, 0:n], in0=t3[:, 0:n], in1=mai[:, 0:n], op=ALU.add),
                             reads=[bt3, bmai], writes=[bmo])
                        S.dma("sp", lambda e, mo=mo, c=c, t0=t0, n=n: e.dma_start(out=mT[c][:, t0:t0 + n], in_=mo[:, 0:n]), reads=[bmo])
                S.barrier()
                S.emit()

        def layer_norm_fm(ph_tiles, z, bz, n, gcol, bcol):
            (sqr, mean, m2, rstd, bst, pssum, pssq) = ph_tiles
            (ps1, bp1), (ps2, bp2) = pssum, pssq
            for c in range(16):
                S.op("pe", lambda e, c=c: e.matmul(ps1[:, 0:n], onesf[:, :], z[:, c * n:(c + 1) * n], start=(c == 0), stop=(c == 15)),
                     reads=[bz, bconst], writes=[bp1], pe_chain=(c > 0))
                sq, bsq = sqr.next()
                S.op("act", lambda e, sq=sq, c=c: e.activation(out=sq[:, 0:n], in_=z[:, c * n:(c + 1) * n], func=AF.Square), reads=[bz], writes=[bsq])
                S.op("pe", lambda e, sq=sq, c=c: e.matmul(ps2[:, 0:n], onesf[:, :], sq[:, 0:n], start=(c == 0), stop=(c == 15)),
                     reads=[bsq, bconst], writes=[bp2], pe_chain=(c > 0))
            S.op("dve", lambda e: e.tensor_scalar(out=mean[:, 0:n], in0=ps1[:, 0:n], scalar1=1.0 / D, scalar2=None, op0=ALU.mult), reads=[bp1], writes=[bst])
            S.op("pool", lambda e: e.tensor_tensor(out=m2[:, 0:n], in0=mean[:, 0:n], in1=mean[:, 0:n], op=ALU.mult), reads=[bst], writes=[bst])
            S.op("dve", lambda e: e.scalar_tensor_tensor(out=rstd[:, 0:n], in0=ps2[:, 0:n], scalar=1.0 / D, in1=m2[:, 0:n], op0=ALU.mult, op1=ALU.subtract),
                 reads=[bp2, bst], writes=[bst])
            S.op("dve", lambda e: e.tensor_scalar(out=rstd[:, 0:n], in0=rstd[:, 0:n], scalar1=LN_EPS, scalar2=None, op0=ALU.add),
                 reads=[bst], writes=[bst])
            S.op("act", lambda e: e.activation(out=rstd[:, 0:n], in_=rstd[:, 0:n], func=AF.Sqrt), reads=[bst], writes=[bst])
            S.op("dve", lambda e: e.reciprocal(out=rstd[:, 0:n], in_=rstd[:, 0:n]), reads=[bst], writes=[bst])
            for c in range(16):
                zc = z[:, c * n:(c + 1) * n]
                S.op("dve", lambda e, zc=zc: e.tensor_tensor(out=zc, in0=zc, in1=mean[:, 0:n], op=ALU.subtract), reads=[bz, bst], writes=[bz])
                S.op("pool", lambda e, zc=zc: e.tensor_tensor(out=zc, in0=zc, in1=rstd[:, 0:n], op=ALU.mult), reads=[bz, bst], writes=[bz])
                S.op("act", lambda e, zc=zc, c=c: e.activation(out=zc, in_=zc, func=AF.Identity, bias=bcol(c), scale=gcol(c)), reads=[bz, bconst], writes=[bz])

        def ln_tiles(ph):
            return (Ring([(sb(ph, f"sq{i}", [128, 512], F32), Buf()) for i in range(2)]),
                    sb(ph, "mean", [128, 512], F32), sb(ph, "m2", [128, 512], F32), sb(ph, "rstd", [128, 512], F32), Buf(),
                    (psb[4], psB[4]), (psb[5], psB[5]))

        def phase_out(l, xres):
            _, groups = layer_groups(l)
            with ExitStack() as ph:
                wo, bwo = load_w2048(ph, "wo", w_o, l)
                lnt_t = ln_tiles(ph)
                msr = Ring([(sb(ph, f"ms{i}", [128, 16 * 512], BF16), Buf()) for i in range(1)])
                xsr = Ring([(sb(ph, f"xs{i}", [128, 16 * 512], F32), Buf()) for i in range(1)])
                h2b = sb(ph, "h2b", [128, 16 * 512], BF16)
                bh2b = Buf()
                psr = Ring([(psb[i], psB[i]) for i in range(3)])
                h2t = sb(ph, "h2t", [128, 2048], BF16)
                bh2t = Buf()
                R = {nm: (sb(ph, "r_" + nm, [128, w], F32), Buf()) for nm, w in
                     (("lg", 32), ("ex", 32), ("pr", 32), ("sel", 32), ("eq", 32), ("sel2", 32), ("oh1", 32), ("oh2", 32), ("tmp", 32),
                      ("m1g", 4), ("m2g", 4), ("gs", 4), ("gh", 4), ("t4", 4), ("sc", 16))}
                cwb = sb(ph, "cwb", [128, 32], BF16)
                bcwb = Buf()
                brt = Buf()
                for (t0, n, isc) in groups:
                    s = 1 if isc else 0
                    ms, bms = msr.next()
                    xs, bxs = xsr.next()
                    S.dma("sp", lambda e, ms=ms, t0=t0, n=n: e.dma_start(out=ms[:, 0:16 * n].rearrange("p (k t) -> p k t", k=16),
                                                                        in_=mT[:, :, t0:t0 + n].rearrange("k p t -> p k t")), writes=[bms])
                    S.dma("sp", lambda e, xs=xs, t0=t0, n=n: e.dma_start(out=xs[:, 0:16 * n].rearrange("p (k t) -> p k t", k=16),
                                                                        in_=xres[:, :, t0:t0 + n].rearrange("k p t -> p k t")), writes=[bxs])
                    for c in range(16):
                        ps, bp = psr.next()
                        for k in range(16):
                            S.op("pe", lambda e, ps=ps, k=k, c=c, ms=ms, n=n: e.matmul(
                                ps[:, 0:n], wo[:, k * 2048 + c * 128:k * 2048 + (c + 1) * 128], ms[:, k * n:(k + 1) * n],
                                start=(k == 0), stop=(k == 15)), reads=[bwo, bms], writes=[bp], pe_chain=(k > 0))
                        xc = xs[:, c * n:(c + 1) * n]
                        S.op("act", lambda e, xc=xc: e.activation(out=xc, in_=xc, func=AF.Copy, scale=ALPHA), reads=[bxs], writes=[bxs])
                        S.op("dve", lambda e, xc=xc, ps=ps, c=c, n=n, s=s: e.scalar_tensor_tensor(
                            out=xc, in0=ps[:, 0:n], scalar=mcol(l, 2, c, s), in1=xc, op0=ALU.mult, op1=ALU.add), reads=[bp, bxs, bmod[l]], writes=[bxs])
                    if "noln" not in dbg:
                        layer_norm_fm(lnt_t, xs, bxs, n,
                                      lambda c: lnt[:, l * 64 + c:l * 64 + c + 1], lambda c: lnt[:, l * 64 + 16 + c:l * 64 + 16 + c + 1])
                    S.dma("sp", lambda e, xs=xs, t0=t0, n=n: e.dma_start(out=x1T[:, :, t0:t0 + n].rearrange("k p t -> p k t"),
                                                                        in_=xs[:, 0:16 * n].rearrange("p (k t) -> p k t", k=16)), reads=[bxs])
                    h2f, bh2f = xs, bxs
                    for c in range(16):
                        S.op("act", lambda e, xs=xs, c=c, n=n, s=s: e.activation(
                            out=xs[:, c * n:(c + 1) * n], in_=xs[:, c * n:(c + 1) * n], func=AF.Identity,
                            bias=mcol(l, 3, c, s), scale=mcol(l, 4, c, s, True)), reads=[bxs, bmod[l]], writes=[bxs])
                        S.op("pool", lambda e, xs=xs, c=c, n=n: e.tensor_copy(out=h2b[:, c * n:(c + 1) * n], in_=xs[:, c * n:(c + 1) * n]), reads=[bxs], writes=[bh2b])
                    S.dma("sp", lambda e, t0=t0, n=n: e.dma_start(out=h2T[:, :, t0:t0 + n].rearrange("k p t -> p k t"),
                                                                 in_=h2b[:, 0:16 * n].rearrange("p (k t) -> p k t", k=16)), reads=[bh2b])
                    if moe == "sorted":
                        for i in range(n // 128):
                            for c in range(16):
                                S.op("pe", lambda e, c=c, i=i, n=n: e.transpose(psbf[c // 8][:, (c % 8) * 128:(c % 8 + 1) * 128],
                                                                                h2b[:, c * n + i * 128:c * n + (i + 1) * 128], identb[:, :]),
                                     reads=[bh2b, bconst], writes=[psB[6 + c // 8]])
                            S.op("act", lambda e: e.activation(out=h2t[:, 0:1024], in_=psbf[0][:, :], func=AF.Copy), reads=[psB[6]], writes=[bh2t])
                            S.op("dve", lambda e: e.tensor_copy(out=h2t[:, 1024:2048], in_=psbf[1][:, :]), reads=[psB[7]], writes=[bh2t])
                            r0 = t0 + i * 128
                            S.dma("sp", lambda e, r0=r0: e.dma_start(out=h2tok[r0:r0 + 128, :], in_=h2t[:, :]), reads=[bh2t])
                    for i in range(0 if 'norouter' in dbg else n // 128):
                        pl, bpl = psb[3], psB[3]
                        for k in range(16):
                            S.op("pe", lambda e, k=k, i=i, n=n, h2f=h2f: e.matmul(pl[:, 0:32], h2f[:, k * n + i * 128:k * n + (i + 1) * 128], rwt[:, k * 32:(k + 1) * 32],
                                                                         start=(k == 0), stop=(k == 15)), reads=[bh2f, bconst], writes=[bpl], pe_chain=(k > 0))
                        T = {k_: v[0] for k_, v in R.items()}
                        sc = T["sc"]

                        rstop = [int(x[5:]) for x in dbg if x.startswith("rstop")]
                        rstop = rstop[0] if rstop else 10 ** 6
                        vcnt = [0]

                        def V(fn, extra_r=()):
                            vcnt[0] += 1
                            if vcnt[0] > rstop:
                                return
                            S.op("dve", fn, reads=[brt, bconst] + list(extra_r), writes=[brt])
                        tid = t0 // 128 + i
                        if moe == "sorted":
                            oh1t = OH[:, tid * 64:tid * 64 + 32]
                            oh2t = OH[:, tid * 64 + 32:tid * 64 + 64]
                        else:
                            oh1t = T["oh1"][:, :]
                            oh2t = T["oh2"][:, :]
                        g3 = lambda t: t[:, 0:32].rearrange("p (g j) -> p g j", g=4)
                        g3a = lambda a: a.rearrange("p (g j) -> p g j", g=4)
                        bc3 = lambda t: t[:, 0:4].unsqueeze(2).to_broadcast([128, 4, 8])
                        V(lambda e: e.tensor_copy(out=T["lg"][:, :], in_=pl[:, 0:32]), [bpl])
                        V(lambda e: e.tensor_reduce(out=sc[:, 0:1], in_=T["lg"][:, :], axis=AX.X, op=ALU.max))
                        V(lambda e: e.tensor_scalar(out=sc[:, 1:2], in0=sc[:, 0:1], scalar1=-1.0, scalar2=None, op0=ALU.mult))
                        if rstop >= 4:
                            S.op("act", lambda e: e.activation(out=T["ex"][:, :], in_=T["lg"][:, :], func=AF.Exp, bias=sc[:, 1:2], scale=1.0),
                                 reads=[brt], writes=[brt])
                        V(lambda e: e.tensor_reduce(out=sc[:, 2:3], in_=T["ex"][:, :], axis=AX.X, op=ALU.add))
                        V(lambda e: e.reciprocal(out=sc[:, 3:4], in_=sc[:, 2:3]))
                        V(lambda e: e.tensor_scalar(out=T["pr"][:, :], in0=T["ex"][:, :], scalar1=sc[:, 3:4], scalar2=None, op0=ALU.mult))
                        V(lambda e: e.tensor_tensor(out=T["sel"][:, :], in0=T["pr"][:, :], in1=rbt[:, :], op=ALU.add))
                        V(lambda e: e.tensor_reduce(out=T["m1g"][:, :], in_=g3(T["sel"]), axis=AX.X, op=ALU.max))
                        V(lambda e: e.tensor_tensor(out=g3(T["eq"]), in0=g3(T["sel"]), in1=bc3(T["m1g"]), op=ALU.is_equal))
                        V(lambda e: e.scalar_tensor_tensor(out=T["sel2"][:, :], in0=T["eq"][:, :], scalar=-1e9, in1=T["sel"][:, :], op0=ALU.mult, op1=ALU.add))
                        V(lambda e: e.tensor_reduce(out=T["m2g"][:, :], in_=g3(T["sel2"]), axis=AX.X, op=ALU.max))
                        V(lambda e: e.tensor_tensor(out=T["gs"][:, :], in0=T["m1g"][:, :], in1=T["m2g"][:, :], op=ALU.add))
                        V(lambda e: e.tensor_reduce(out=sc[:, 4:5], in_=T["gs"][:, :], axis=AX.X, op=ALU.max))
                        V(lambda e: e.tensor_tensor(out=T["gh"][:, :], in0=T["gs"][:, :], in1=sc[:, 4:5].to_broadcast([128, 4]), op=ALU.is_equal))
                        V(lambda e: e.tensor_tensor(out=T["t4"][:, :], in0=T["gh"][:, :], in1=T["m1g"][:, :], op=ALU.mult))
                        V(lambda e: e.tensor_reduce(out=sc[:, 5:6], in_=T["t4"][:, :], axis=AX.X, op=ALU.add))
                        V(lambda e: e.tensor_tensor(out=T["t4"][:, :], in0=T["gh"][:, :], in1=T["m2g"][:, :], op=ALU.mult))
                        V(lambda e: e.tensor_reduce(out=sc[:, 6:7], in_=T["t4"][:, :], axis=AX.X, op=ALU.add))
                        V(lambda e, oh1t=oh1t, oh2t=oh2t: e.tensor_tensor(out=oh1t, in0=T["sel"][:, :], in1=sc[:, 5:6].to_broadcast([128, 32]), op=ALU.is_equal))
                        V(lambda e, oh1t=oh1t, oh2t=oh2t: e.tensor_tensor(out=g3a(oh1t), in0=g3a(oh1t), in1=bc3(T["gh"]), op=ALU.mult))
                        V(lambda e, oh1t=oh1t, oh2t=oh2t: e.tensor_tensor(out=oh2t, in0=T["sel"][:, :], in1=sc[:, 6:7].to_broadcast([128, 32]), op=ALU.is_equal))
                        V(lambda e, oh1t=oh1t, oh2t=oh2t: e.tensor_tensor(out=g3a(oh2t), in0=g3a(oh2t), in1=bc3(T["gh"]), op=ALU.mult))
                        V(lambda e, oh1t=oh1t, oh2t=oh2t: e.tensor_tensor(out=T["tmp"][:, :], in0=oh1t, in1=T["pr"][:, :], op=ALU.mult))
                        V(lambda e: e.tensor_reduce(out=sc[:, 7:8], in_=T["tmp"][:, :], axis=AX.X, op=ALU.add))
                        V(lambda e, oh1t=oh1t, oh2t=oh2t: e.tensor_tensor(out=T["tmp"][:, :], in0=oh2t, in1=T["pr"][:, :], op=ALU.mult))
                        V(lambda e: e.tensor_reduce(out=sc[:, 8:9], in_=T["tmp"][:, :], axis=AX.X, op=ALU.add))
                        V(lambda e: e.tensor_tensor(out=sc[:, 9:10], in0=sc[:, 7:8], in1=sc[:, 8:9], op=ALU.add))
                        V(lambda e: e.reciprocal(out=sc[:, 10:11], in_=sc[:, 9:10]))
                        V(lambda e: e.tensor_tensor(out=sc[:, 11:12], in0=sc[:, 7:8], in1=sc[:, 10:11], op=ALU.mult))
                        V(lambda e: e.tensor_tensor(out=sc[:, 12:13], in0=sc[:, 8:9], in1=sc[:, 10:11], op=ALU.mult))
                        if moe == "sorted":
                            S.op("dve", lambda e, tid=tid: e.tensor_copy(out=WW[:, 2 * tid:2 * tid + 2], in_=sc[:, 11:13]), reads=[brt], writes=[brt, brout])
                            continue
                        V(lambda e: e.tensor_scalar(out=T["tmp"][:, :], in0=T["oh1"][:, :], scalar1=sc[:, 11:12], scalar2=None, op0=ALU.mult))
                        if rstop < 10 ** 6:
                            continue
                        S.op("dve", lambda e: e.scalar_tensor_tensor(out=cwb[:, :], in0=T["oh2"][:, :], scalar=sc[:, 12:13], in1=T["tmp"][:, :], op0=ALU.mult, op1=ALU.add),
                             reads=[brt], writes=[bcwb])
                        ptp, bptp = psb[7], psB[7]
                        S.op("pe", lambda e: e.matmul(ptp[0:32, 0:128], cwb[:, :], identb[:, :], start=True, stop=True), reads=[bcwb, bconst], writes=[bptp])
                        tc0 = t0 + i * 128
                        S.op("act", lambda e, tc0=tc0: e.activation(out=cwT[:, tc0:tc0 + 128], in_=ptp[0:32, 0:128], func=AF.Copy), reads=[bptp], writes=[bcwT])
                if "cwTd" in taps:
                    S.dma("sp", lambda e: e.dma_start(out=cwTd[:, :], in_=cwT[:, :]), reads=[bcwT])
                S.barrier()
                S.emit()

        def phase_moe(l):
            _, groups = layer_groups(l)
            with ExitStack() as ph:
                h2r = Ring([(sb(ph, f"h2q{i}", [128, 16 * 512], BF16), Buf()) for i in range(1)])
                far = Ring([(sb(ph, f"fa{i}", [128, 16 * 512], F32), Buf()) for i in range(1)])
                wgr = Ring([(sb(ph, f"wg{i}", [128, 16 * 512], BF16), Buf()) for i in range(2)])
                wur = Ring([(sb(ph, f"wu{i}", [128, 16 * 512], BF16), Buf()) for i in range(2)])
                wdr = Ring([(sb(ph, f"wd{i}", [128, 4 * 2048], BF16), Buf()) for i in range(2)])
                cwr = Ring([(sb(ph, f"cws{i}", [128, 512], F32), Buf()) for i in range(2)])
                sgr = Ring([(sb(ph, f"sg{i}", [128, 512], F32), Buf()) for i in range(3)])
                tr = Ring([(sb(ph, f"tt{i}", [128, 512], F32), Buf()) for i in range(3)])
                acr = Ring([(sb(ph, f"ac{i}", [128, 4 * 512], BF16), Buf()) for i in range(2)])
                psg = Ring([(psb[i], psB[i]) for i in (0, 1)])
                psu = Ring([(psb[i], psB[i]) for i in (2, 3)])
                psy = Ring([(psb[i], psB[i]) for i in (4, 5, 6)])
                pbc, bpbc = psb[7], psB[7]
                for (t0, n, isc) in groups:
                    h2q, bh2 = h2r.next()
                    fa, bfa = far.next()
                    S.dma("sp", lambda e, h2q=h2q, t0=t0, n=n: e.dma_start(out=h2q[:, 0:16 * n].rearrange("p (k t) -> p k t", k=16),
                                                                          in_=h2T[:, :, t0:t0 + n].rearrange("k p t -> p k t")), writes=[bh2])
                    for ex in range(NE):
                        cws, bcws = cwr.next()
                        S.op("pe", lambda e, ex=ex, t0=t0, n=n: e.matmul(pbc[:, 0:n], selEb[0:32, ex * 128:(ex + 1) * 128], cwT[0:32, t0:t0 + n], start=True, stop=True),
                             reads=[bcwT, bconst], writes=[bpbc])
                        S.op("act", lambda e, cws=cws, n=n: e.activation(out=cws[:, 0:n], in_=pbc[:, 0:n], func=AF.Copy), reads=[bpbc], writes=[bcws])
                        for half in range(2):
                            wg, bwg = wgr.next()
                            wu, bwu = wur.next()
                            wd, bwd = wdr.next()
                            f0 = half * 512
                            S.dma("pool", lambda e, wg=wg, ex=ex, f0=f0: e.dma_start(out=wg[:, :].rearrange("p (k f) -> p k f", k=16),
                                                                                  in_=w_g[l, ex, :, f0:f0 + 512].rearrange("(k p) f -> p k f", p=128)), writes=[bwg])
                            S.dma("pool", lambda e, wu=wu, ex=ex, f0=f0: e.dma_start(out=wu[:, :].rearrange("p (k f) -> p k f", k=16),
                                                                                  in_=w_u[l, ex, :, f0:f0 + 512].rearrange("(k p) f -> p k f", p=128)), writes=[bwu])
                            S.dma("pool", lambda e, wd=wd, ex=ex, f0=f0: e.dma_start(out=wd[:, :].rearrange("p (j c) -> p j c", j=4),
                                                                                  in_=w_d[l, ex, f0:f0 + 512, :].rearrange("(j p) c -> p j c", p=128)), writes=[bwd])
                            ac, bac = acr.next()
                            for jf in range(4):
                                pg, bpg = psg.next()
                                pu, bpu = psu.next()
                                for k in range(16):
                                    S.op("pe", lambda e, pg=pg, wg=wg, k=k, jf=jf, h2q=h2q, n=n: e.matmul(
                                        pg[:, 0:n], wg[:, k * 512 + jf * 128:k * 512 + (jf + 1) * 128], h2q[:, k * n:(k + 1) * n],
                                        start=(k == 0), stop=(k == 15)), reads=[bwg, bh2], writes=[bpg], pe_chain=(k > 0))
                                for k in range(16):
                                    S.op("pe", lambda e, pu=pu, wu=wu, k=k, jf=jf, h2q=h2q, n=n: e.matmul(
                                        pu[:, 0:n], wu[:, k * 512 + jf * 128:k * 512 + (jf + 1) * 128], h2q[:, k * n:(k + 1) * n],
                                        start=(k == 0), stop=(k == 15)), reads=[bwu, bh2], writes=[bpu], pe_chain=(k > 0))
                                sg, bsg = sgr.next()
                                S.op("act", lambda e, sg=sg, pg=pg, n=n: e.activation(out=sg[:, 0:n], in_=pg[:, 0:n], func=AF.Silu), reads=[bpg], writes=[bsg])
                                tt, btt = tr.next()
                                S.op("dve", lambda e, tt=tt, sg=sg, pu=pu, n=n: e.tensor_tensor(out=tt[:, 0:n], in0=pu[:, 0:n], in1=sg[:, 0:n], op=ALU.mult),
                                     reads=[bpu, bsg], writes=[btt])
                                S.op("dve", lambda e, ac=ac, jf=jf, tt=tt, cws=cws, n=n: e.tensor_tensor(out=ac[:, jf * n:(jf + 1) * n], in0=tt[:, 0:n], in1=cws[:, 0:n], op=ALU.mult),
                                     reads=[btt, bcws], writes=[bac])
                            for c in range(16):
                                py, bpy = psy.next()
                                for jf in range(4):
                                    S.op("pe", lambda e, py=py, wd=wd, jf=jf, c=c, ac=ac, n=n: e.matmul(
                                        py[:, 0:n], wd[:, jf * 2048 + c * 128:jf * 2048 + (c + 1) * 128], ac[:, jf * n:(jf + 1) * n],
                                        start=(jf == 0), stop=(jf == 3)), reads=[bwd, bac], writes=[bpy], pe_chain=(jf > 0))
                                fc = fa[:, c * n:(c + 1) * n]
                                if ex == 0 and half == 0:
                                    S.op("dve", lambda e, fc=fc, py=py, n=n: e.tensor_copy(out=fc, in_=py[:, 0:n]), reads=[bpy], writes=[bfa])
                                else:
                                    S.op("dve", lambda e, fc=fc, py=py, n=n: e.tensor_tensor(out=fc, in0=fc, in1=py[:, 0:n], op=ALU.add), reads=[bpy, bfa], writes=[bfa])
                    S.dma("sp", lambda e, fa=fa, t0=t0, n=n: e.dma_start(out=fT[:, :, t0:t0 + n].rearrange("k p t -> p k t"),
                                                                        in_=fa[:, 0:16 * n].rearrange("p (k t) -> p k t", k=16)), reads=[bfa])
                S.barrier()
                S.emit()


        def layer_tiles(l):
            _, groups = layer_groups(l)
            return [t0 // 128 + i for (t0, n, isc) in groups for i in range(n // 128)]

        def n_blocks(l):
            return (len(layer_tiles(l)) * 256) // 128 + NE

        def phase_slots(l):
            tiles = layer_tiles(l)
            NB = n_blocks(l)
            with ExitStack() as ph:
                cum = sb(ph, "cum", [128, 32], F32)
                cumb = sb(ph, "cumb", [128, 32], BF16)
                mf = sb(ph, "mf", [128, 32], F32)
                mb = sb(ph, "mb", [128, 32], BF16)
                cnt = sb(ph, "cnt", [128, 32], F32)
                big = sb(ph, "big", [128, 104 * 32], F32)
                nbk = sb(ph, "nbk", [128, 32], F32)
                pst = sb(ph, "pst", [128, 33], F32)
                bef = sb(ph, "bef", [128, 128], F32)
                chf = sb(ph, "chf", [128, 128], F32)
                dst = sb(ph, "dst", [128, 32], F32)
                tm = sb(ph, "tm", [128, 32], F32)
                sf = sb(ph, "sf", [128, 2], F32)
                b1 = Buf()
                pp, bpp = psb[0], psB[0]

                def V(fn, extra_r=(), extra_w=()):
                    S.op("dve", fn, reads=[b1, bconst, brout] + list(extra_r), writes=[b1] + list(extra_w))
                V(lambda e: e.memset(cum[:, :], 0.0))
                for tid in tiles:
                    o1 = OH[:, tid * 64:tid * 64 + 32]
                    o2 = OH[:, tid * 64 + 32:tid * 64 + 64]
                    V(lambda e, o1=o1, o2=o2: e.tensor_tensor(out=mf[:, :], in0=o1, in1=o2, op=ALU.add))
                    V(lambda e: e.tensor_copy(out=mb[:, :], in_=mf[:, :]))
                    V(lambda e: e.tensor_copy(out=cumb[:, :], in_=cum[:, :]))
                    S.op("pe", lambda e: e.matmul(pp[:, 0:32], ustr[:, :], mb[:, :], start=True, stop=False), reads=[b1, bconst], writes=[bpp])
                    S.op("pe", lambda e: e.matmul(pp[:, 0:32], onesb[:, :], cumb[:, :], start=False, stop=True), reads=[b1, bconst], writes=[bpp], pe_chain=True)
                    V(lambda e, tid=tid: e.tensor_copy(out=POS[:, tid * 32:(tid + 1) * 32], in_=pp[:, 0:32]), [bpp])
                    V(lambda e: e.tensor_tensor(out=cum[:, :], in0=cum[:, :], in1=mf[:, :], op=ALU.add))
                V(lambda e: e.tensor_copy(out=cumb[:, :], in_=cum[:, :]))
                S.op("pe", lambda e: e.matmul(pp[:, 0:32], onesb[:, :], cumb[:, :], start=True, stop=True), reads=[b1, bconst], writes=[bpp])
                V(lambda e: e.tensor_copy(out=cnt[:, :], in_=pp[:, 0:32]), [bpp])
                J = 80
                V(lambda e: e.tensor_tensor(out=big[:, 0:32 * J].rearrange("p (a j) -> p a j", a=32),
                                            in0=cnt[:, :].unsqueeze(2).to_broadcast([128, 32, J]),
                                            in1=thr[:, 0:J].unsqueeze(1).to_broadcast([128, 32, J]), op=ALU.is_gt))
                V(lambda e: e.tensor_reduce(out=nbk[:, :], in_=big[:, 0:32 * J].rearrange("p (a j) -> p a j", a=32), axis=AX.X, op=ALU.add))
                V(lambda e: e.tensor_scalar(out=nbk[:, :], in0=nbk[:, :], scalar1=128.0, scalar2=None, op0=ALU.mult))
                V(lambda e: e.memset(pst[:, 0:1], 0.0))
                for ex in range(32):
                    V(lambda e, ex=ex: e.tensor_tensor(out=pst[:, ex + 1:ex + 2], in0=pst[:, ex:ex + 1], in1=nbk[:, ex:ex + 1], op=ALU.add))
                V(lambda e: e.tensor_tensor(out=big[:, 0:NB * 32].rearrange("p (b a) -> p b a", a=32),
                                            in0=pst[:, 1:33].unsqueeze(1).to_broadcast([128, NB, 32]),
                                            in1=thr[:, 0:NB].unsqueeze(2).to_broadcast([128, NB, 32]), op=ALU.is_le))
                V(lambda e: e.tensor_reduce(out=bef[:, 0:NB], in_=big[:, 0:NB * 32].rearrange("p (b a) -> p b a", a=32), axis=AX.X, op=ALU.add))
                V(lambda e: e.tensor_scalar(out=bef[:, 0:NB], in0=bef[:, 0:NB], scalar1=31.0, scalar2=None, op0=ALU.min))
                V(lambda e: e.tensor_copy(out=BLKE[:, 0:NB], in_=bef[:, 0:NB]), extra_w=[bblk])
                V(lambda e: e.memset(chf[:, 0:1], 1.0))
                V(lambda e: e.tensor_tensor(out=chf[:, 1:NB], in0=bef[:, 1:NB], in1=bef[:, 0:NB - 1], op=ALU.not_equal))
                V(lambda e: e.tensor_copy(out=CHG[:, 0:NB], in_=chf[:, 0:NB]), extra_w=[bblk])
                idf = sb(ph, "idf", [128, 128], F32)
                pen = sb(ph, "pen", [128, 128], F32)
                V(lambda e: e.tensor_scalar(out=pen[:, 0:NB], in0=chf[:, 0:NB], scalar1=-1.0, scalar2=-4.0e6, op0=ALU.add, op1=ALU.mult))
                for half in range(2):
                    for j in range(4):
                        V(lambda e, half=half, j=j: e.tensor_scalar(out=idf[:, 0:NB], in0=bef[:, 0:NB], scalar1=1024.0,
                                                                    scalar2=float((l * NE * 256 + half * 128) * 4 + j), op0=ALU.mult, op1=ALU.add))
                        V(lambda e: e.scalar_tensor_tensor(out=idf[:, 0:NB], in0=thr[:, 128:129].to_broadcast([128, NB]), scalar=4.0, in1=idf[:, 0:NB], op0=ALU.mult, op1=ALU.add))
                        V(lambda e: e.tensor_tensor(out=idf[:, 0:NB], in0=idf[:, 0:NB], in1=pen[:, 0:NB], op=ALU.add))
                        V(lambda e, half=half, j=j: e.tensor_copy(out=IDXT[:, (half * 4 + j) * 128:(half * 4 + j) * 128 + NB], in_=idf[:, 0:NB]), extra_w=[bblk])
                for tid in tiles:
                    V(lambda e, tid=tid: e.tensor_tensor(out=dst[:, :], in0=POS[:, tid * 32:(tid + 1) * 32], in1=pst[:, 0:32], op=ALU.add))
                    for j in range(2):
                        oh = OH[:, tid * 64 + j * 32:tid * 64 + (j + 1) * 32]
                        V(lambda e, oh=oh: e.tensor_tensor(out=tm[:, :], in0=dst[:, :], in1=oh, op=ALU.mult))
                        V(lambda e, j=j: e.tensor_reduce(out=sf[:, j:j + 1], in_=tm[:, :], axis=AX.X, op=ALU.add))
                    V(lambda e, tid=tid: e.tensor_copy(out=SLOT[:, 2 * tid:2 * tid + 2], in_=sf[:, :]), extra_w=[bslot])
                if "slotd" in taps:
                    S.dma("sp", lambda e: e.dma_start(out=slotd[:, :], in_=SLOT[:, :]), reads=[bslot])
                    S.dma("sp", lambda e: e.dma_start(out=blked[:, 0:128], in_=BLKE[:, :]), reads=[bblk])
                    S.dma("sp", lambda e: e.dma_start(out=blked[:, 128:256], in_=IDXT[:, 128:256]), reads=[bblk])
                S.barrier()
                S.emit()

        def phase_dispatch(l):
            tiles = layer_tiles(l)
            with ExitStack() as ph:
                htr = Ring([(sb(ph, f"ht{i}", [128, 2048], BF16), Buf()) for i in range(3)])
                for tid in tiles:
                    ht, bht = htr.next()
                    S.dma("sp", lambda e, ht=ht, tid=tid: e.dma_start(out=ht[:, :], in_=h2tok[tid * 128:(tid + 1) * 128, :]), writes=[bht])
                    for j in range(2):
                        S.dma("pool", lambda e, ht=ht, tid=tid, j=j: e.indirect_dma_start(
                            out=xsd[:, :], out_offset=bass.IndirectOffsetOnAxis(ap=SLOT[:, 2 * tid + j:2 * tid + j + 1], axis=0),
                            in_=ht[:, :], in_offset=None), reads=[bht, bslot])
                S.barrier()
                S.emit()

        regs = {}

        def phase_experts(l):
            NB = n_blocks(l)
            with ExitStack() as ph:
                wg = [(sb(ph, f"wgh{h}", [128, 16 * 512], BF16), [Buf() for _ in range(4)]) for h in range(2)]
                wu = [(sb(ph, f"wuh{h}", [128, 16 * 512], BF16), [Buf() for _ in range(4)]) for h in range(2)]
                wd = [(sb(ph, f"wdh{h}", [128, 4 * 2048], BF16), [Buf() for _ in range(4)]) for h in range(2)]
                xbr = Ring([(sb(ph, f"xb{i}", [128, 2048], BF16), Buf()) for i in range(2)])
                xtr = Ring([(sb(ph, f"xt{i}", [128, 2048], BF16), Buf()) for i in range(2)])
                sgr = Ring([(sb(ph, f"sg{i}", [128, 512], F32), Buf()) for i in range(2)])
                acr = Ring([(sb(ph, f"ac{i}", [128, 512], BF16), Buf()) for i in range(2)])
                atr = Ring([(sb(ph, f"act{i}", [128, 512], BF16), Buf()) for i in range(2)])
                ybr = Ring([(sb(ph, f"yb{i}", [128, 2048], F32), Buf()) for i in range(2)])
                def wgather(b, wsrc, wt, half):
                    for j in range(4):
                        def fn(e, j=j):
                            if "bv" not in regs:
                                reg = e.register("r_bound").__enter__()
                                e.reg_mov(reg, NROWS * 4 - 1)
                                regs["bv"] = e.snap(reg)
                            return e.indirect_dma_start(
                                out=wt[0][:, j * 2048:(j + 1) * 2048], out_offset=None, in_=wsrc[:, :],
                                in_offset=bass.IndirectOffsetOnAxis(ap=IDXT[:, (half * 4 + j) * 128 + b:(half * 4 + j) * 128 + b + 1], axis=0),
                                bounds_check=regs["bv"], oob_is_err=False)
                        S.dma("pool", fn, reads=[bblk], writes=[wt[1][j]])
                for b in range(NB):
                    for half in range(2):
                        wgather(b, w_g, wg[half], half)
                        wgather(b, w_u, wu[half], half)
                        wgather(b, w_d, wd[half], half)
                    xb, bxb = xbr.next()
                    S.dma("sp", lambda e, xb=xb, b=b: e.dma_start(out=xb[:, :], in_=xsd[b * 128:(b + 1) * 128, :]), writes=[bxb])
                    for k in range(16):
                        S.op("pe", lambda e, k=k, xb=xb: e.transpose(psbf[k // 8][:, (k % 8) * 128:(k % 8 + 1) * 128], xb[:, k * 128:(k + 1) * 128], identb[:, :]),
                             reads=[bxb, bconst], writes=[psB[6 + k // 8]])
                    xt, bxt = xtr.next()
                    S.op("act", lambda e, xt=xt: e.activation(out=xt[:, 0:1024], in_=psbf[0][:, :], func=AF.Copy), reads=[psB[6]], writes=[bxt])
                    S.op("dve", lambda e, xt=xt: e.tensor_copy(out=xt[:, 1024:2048], in_=psbf[1][:, :]), reads=[psB[7]], writes=[bxt])
                    for half in range(2):
                        for (wt, pi) in ((wg[half], 0), (wu[half], 1)):
                            for k in range(16):
                                S.op("pe", lambda e, wt=wt, pi=pi, k=k, xt=xt: e.matmul(psb[pi][:, 0:512], xt[:, k * 128:(k + 1) * 128], wt[0][:, k * 512:(k + 1) * 512],
                                                                                   start=(k == 0), stop=(k == 15)), reads=[bxt, wt[1][k // 4]], writes=[psB[pi]], pe_chain=(k > 0))
                        sg, bsg = sgr.next()
                        S.op("act", lambda e, sg=sg: e.activation(out=sg[:, :], in_=psb[0][:, 0:512], func=AF.Silu), reads=[psB[0]], writes=[bsg])
                        ac, bac = acr.next()
                        S.op("dve", lambda e, ac=ac, sg=sg: e.tensor_tensor(out=ac[:, :], in0=psb[1][:, 0:512], in1=sg[:, :], op=ALU.mult), reads=[psB[1], bsg], writes=[bac])
                        for jf in range(4):
                            S.op("pe", lambda e, jf=jf, ac=ac: e.transpose(psbf[0][:, jf * 128:(jf + 1) * 128], ac[:, jf * 128:(jf + 1) * 128], identb[:, :]),
                                 reads=[bac, bconst], writes=[psB[6]])
                        at, bat = atr.next()
                        S.op("act", lambda e, at=at: e.activation(out=at[:, :], in_=psbf[0][:, 0:512], func=AF.Copy), reads=[psB[6]], writes=[bat])
                        for c in range(4):
                            for jf in range(4):
                                first = (half == 0 and jf == 0)
                                last = (half == 1 and jf == 3)
                                S.op("pe", lambda e, c=c, jf=jf, at=at, half=half, first=first, last=last: e.matmul(
                                    psb[2 + c][:, 0:512], at[:, jf * 128:(jf + 1) * 128], wd[half][0][:, jf * 2048 + c * 512:jf * 2048 + (c + 1) * 512],
                                    start=first, stop=last), reads=[bat, wd[half][1][jf]], writes=[psB[2 + c]], pe_chain=(not first))
                    yb, byb = ybr.next()
                    for c in range(4):
                        if c % 2 == 0:
                            S.op("act", lambda e, yb=yb, c=c: e.activation(out=yb[:, c * 512:(c + 1) * 512], in_=psb[2 + c][:, 0:512], func=AF.Copy), reads=[psB[2 + c]], writes=[byb])
                        else:
                            S.op("dve", lambda e, yb=yb, c=c: e.tensor_copy(out=yb[:, c * 512:(c + 1) * 512], in_=psb[2 + c][:, 0:512]), reads=[psB[2 + c]], writes=[byb])
                    S.dma("sp", lambda e, yb=yb, b=b: e.dma_start(out=ysd[b * 128:(b + 1) * 128, :], in_=yb[:, :]), reads=[byb])
                S.barrier()
                S.emit()

        def phase_combine(l):
            tiles = layer_tiles(l)
            with ExitStack() as ph:
                ar = Ring([(sb(ph, f"ga{i}", [128, 2048], F32), Buf()) for i in range(2)])
                br_ = Ring([(sb(ph, f"gb{i}", [128, 2048], F32), Buf()) for i in range(2)])
                fbr = Ring([(sb(ph, f"fb{i}", [128, 2048], BF16), Buf()) for i in range(2)])
                ftr = Ring([(sb(ph, f"ft{i}", [128, 2048], F32), Buf()) for i in range(2)])
                for tid in tiles:
                    a, ba = ar.next()
                    bb, bbb = br_.next()
                    S.dma("pool", lambda e, a=a, tid=tid: e.indirect_dma_start(
                        out=a[:, :], out_offset=None, in_=ysd[:, :], in_offset=bass.IndirectOffsetOnAxis(ap=SLOT[:, 2 * tid:2 * tid + 1], axis=0)),
                        reads=[bslot], writes=[ba])
                    S.dma("pool", lambda e, bb=bb, tid=tid: e.indirect_dma_start(
                        out=bb[:, :], out_offset=None, in_=ysd[:, :], in_offset=bass.IndirectOffsetOnAxis(ap=SLOT[:, 2 * tid + 1:2 * tid + 2], axis=0)),
                        reads=[bslot], writes=[bbb])
                    S.op("act", lambda e, a=a, tid=tid: e.activation(out=a[:, :], in_=a[:, :], func=AF.Identity, scale=WW[:, 2 * tid:2 * tid + 1]), reads=[ba, brout], writes=[ba])
                    fb, bfb = fbr.next()
                    S.op("dve", lambda e, fb=fb, a=a, bb=bb, tid=tid: e.scalar_tensor_tensor(out=fb[:, :], in0=bb[:, :], scalar=WW[:, 2 * tid + 1:2 * tid + 2], in1=a[:, :],
                                                                                       op0=ALU.mult, op1=ALU.add), reads=[ba, bbb, brout], writes=[bfb])
                    for k in range(16):
                        S.op("pe", lambda e, k=k, fb=fb: e.transpose(psbf[k // 8][:, (k % 8) * 128:(k % 8 + 1) * 128], fb[:, k * 128:(k + 1) * 128], identb[:, :]),
                             reads=[bfb, bconst], writes=[psB[6 + k // 8]])
                    ft, bft = ftr.next()
                    S.op("act", lambda e, ft=ft: e.activation(out=ft[:, 0:1024], in_=psbf[0][:, :], func=AF.Copy), reads=[psB[6]], writes=[bft])
                    S.op("dve", lambda e, ft=ft: e.tensor_copy(out=ft[:, 1024:2048], in_=psbf[1][:, :]), reads=[psB[7]], writes=[bft])
                    S.dma("sp", lambda e, ft=ft, tid=tid: e.dma_start(out=fT[:, :, tid * 128:(tid + 1) * 128].rearrange("k p t -> p k t"),
                                                                     in_=ft[:, :].rearrange("p (k t) -> p k t", k=16)), reads=[bft])
                S.barrier()
                S.emit()

        def phase_ln2(l):
            _, groups = layer_groups(l)
            with ExitStack() as ph:
                lnt_t = ln_tiles(ph)
                fsr = Ring([(sb(ph, f"fs{i}", [128, 16 * 512], F32), Buf()) for i in range(2)])
                xsr = Ring([(sb(ph, f"xs{i}", [128, 16 * 512], F32), Buf()) for i in range(2)])
                for (t0, n, isc) in groups:
                    s = 1 if isc else 0
                    fs, bfs = fsr.next()
                    xs, bxs = xsr.next()
                    S.dma("sp", lambda e, fs=fs, t0=t0, n=n: e.dma_start(out=fs[:, 0:16 * n].rearrange("p (k t) -> p k t", k=16),
                                                                        in_=fT[:, :, t0:t0 + n].rearrange("k p t -> p k t")), writes=[bfs])
                    S.dma("sp", lambda e, xs=xs, t0=t0, n=n: e.dma_start(out=xs[:, 0:16 * n].rearrange("p (k t) -> p k t", k=16),
                                                                        in_=x1T[:, :, t0:t0 + n].rearrange("k p t -> p k t")), writes=[bxs])
                    for c in range(16):
                        xc = xs[:, c * n:(c + 1) * n]
                        S.op("act", lambda e, xc=xc: e.activation(out=xc, in_=xc, func=AF.Copy, scale=ALPHA), reads=[bxs], writes=[bxs])
                        S.op("dve", lambda e, xc=xc, fs=fs, c=c, n=n, s=s: e.scalar_tensor_tensor(
                            out=xc, in0=fs[:, c * n:(c + 1) * n], scalar=mcol(l, 5, c, s), in1=xc, op0=ALU.mult, op1=ALU.add), reads=[bfs, bxs, bmod[l]], writes=[bxs])
                    layer_norm_fm(lnt_t, xs, bxs, n,
                                  lambda c: lnt[:, l * 64 + 32 + c:l * 64 + 32 + c + 1], lambda c: lnt[:, l * 64 + 48 + c:l * 64 + 48 + c + 1])
                    if l == nlayers - 1 and nlayers == 2:
                        dst = yout[:, :, t0 - HALO:t0 - HALO + n]
                    else:
                        dst = x2T[:, :, t0:t0 + n]
                    S.dma("sp", lambda e, xs=xs, dst=dst, n=n: e.dma_start(out=dst.rearrange("k p t -> p k t"),
                                                                         in_=xs[:, 0:16 * n].rearrange("p (k t) -> p k t", k=16)), reads=[bxs])
                S.barrier()
                S.emit()

        S.emit()
        tmpst.close()
        step = 0
        for l in range(nlayers):
            phase_mod(l)
        for l in range(nlayers):
            xres = xin if l == 0 else x2T
            if moe == "dense":
                fns = (lambda: phase_inproj(l, xres), lambda: phase_attn(l), lambda: phase_conv(l),
                       lambda: phase_out(l, xres), lambda: phase_moe(l), lambda: phase_ln2(l))
            else:
                fns = (lambda: phase_inproj(l, xres), lambda: phase_attn(l), lambda: phase_conv(l),
                       lambda: phase_out(l, xres), lambda: phase_slots(l), lambda: phase_dispatch(l),
                       lambda: phase_experts(l), lambda: phase_combine(l), lambda: phase_ln2(l))
            for fn in fns:
                step += 1
                if step <= upto:
                    fn()
        S.barrier()
        S.emit()
    return nc


def _consts():
    ident = np.eye(128, dtype=np.float32)
    m = np.arange(128)
    partner = np.where((m % 64) < 32, m + 32, m - 32)
    pswap = np.zeros((128, 128), np.float32)
    pswap[partner, m] = 1.0
    kk = np.arange(128)[:, None]
    qq = np.arange(128)[None, :]
    triL = np.tile((kk >= qq).astype(np.float32), (1, 4))
    triU = np.tile((kk <= qq).astype(np.float32), (1, 4))
    sel = np.zeros((128, 32 * 128), np.float32)
    for e in range(32):
        sel[e, e * 128:(e + 1) * 128] = 1.0
    return np.concatenate([ident, pswap, triL, triU, sel], axis=1)


def _fm(a):
    return np.ascontiguousarray(a.T.reshape(KC, 128, a.shape[0]))


def _pk(v):
    return np.ascontiguousarray(v.reshape(KC, 128).T)


def relayout_experts(w_gate, w_up, w_down):
    def gu(w):
        return np.ascontiguousarray(w.reshape(2, NE, 16, 128, 2, 512).transpose(0, 1, 4, 3, 2, 5)).reshape(2 * NE * 2 * 128 * 4, 2048)
    wd = np.ascontiguousarray(w_down.reshape(2, NE, 2, 4, 128, D).transpose(0, 1, 2, 4, 3, 5)).reshape(2 * NE * 2 * 128 * 4, 2048)
    return dict(w_gate=gu(w_gate), w_up=gu(w_up), w_down=wd)


def prep_core(core, x, c, ctx, c_ctx, b_ada, attn_sink, conv_w, ln1_g, ln1_b, ln2_g, ln2_b, router_w, router_b):
    b = core // 4
    s = (core % 4) * OWN
    pos = np.arange(s - HALO, s + OWN + HALO)
    valid = (pos >= 0) & (pos < 16384)
    xl = np.zeros((TL, D), np.float32)
    xl[valid] = x[b, pos[valid]]
    xin = np.concatenate([_fm(xl), _fm(ctx[b])], axis=2)
    cvec = np.stack([_pk(c[b]), _pk(c_ctx)], axis=2).reshape(128, 32)
    badaT = np.concatenate([np.ascontiguousarray(b_ada[l].reshape(96, 128).T) for l in range(2)], axis=1)
    lnp = np.concatenate([_pk(v[l]) for l in range(2) for v in (ln1_g, ln1_b, ln2_g, ln2_b)], axis=1)
    cwt = np.concatenate([np.stack([_pk(conv_w[l, j]) for j in range(3)], axis=2).reshape(128, 48) for l in range(2)], axis=1)
    sinkrow = np.repeat(attn_sink, 128, axis=1).astype(np.float32)
    rw = np.ascontiguousarray(router_w.reshape(KC, 128, NE).transpose(1, 0, 2)).reshape(128, 512)
    rb = np.tile(router_b[None, :], (128, 1)).astype(np.float32)
    posc = np.clip(pos, 0, 16383)
    row = (posc // 64).astype(np.float32)
    col = (posc % 64).astype(np.float32)
    inv = np.power(np.float32(10000.0), -np.arange(32, dtype=np.float32) / np.float32(32)).astype(np.float32)
    m = np.arange(128)
    axis = m // 64
    f = m % 32
    sign = np.where((m % 64) < 32, -1.0, 1.0).astype(np.float32)
    p_ax = np.where(axis[:, None] == 0, row[None, :], col[None, :]).astype(np.float32)
    ang = (p_ax * inv[f][:, None]).astype(np.float32)
    cosT = np.cos(ang).astype(np.float32)
    sinT = (np.sin(ang) * sign[:, None]).astype(np.float32)
    vb = np.where(valid, 0.0, -30000.0).astype(np.float32).reshape(36, 128).T
    vrow = np.tile(valid.astype(np.float32)[None, :], (128, 1))
    return dict(xin=xin, cvec=np.ascontiguousarray(cvec), badaT=np.ascontiguousarray(badaT), lnp=np.ascontiguousarray(lnp),
                cwt=np.ascontiguousarray(cwt), sinkrow=sinkrow, rw=rw, rb=rb, cosT=cosT, sinT=sinT,
                vbias=np.ascontiguousarray(vb), vrow=np.ascontiguousarray(vrow), consts=_consts(),
                thr=np.concatenate([np.tile((np.arange(128, dtype=np.float32) * 128.0)[None, :], (128, 1)), np.arange(128, dtype=np.float32)[:, None]], axis=1))


def kernel(x, c, ctx, c_ctx, w_ada, b_ada, w_in, attn_sink, conv_w, w_attn_proj, w_conv_proj, w_out,
           ln1_g, ln1_b, ln2_g, ln2_b, router_w, router_b, w_gate, w_up, w_down):
    g = lambda a: np.asarray(a, dtype=np.float32)
    x, c, ctx, c_ctx, b_ada, attn_sink, conv_w = g(x), g(c), g(ctx), g(c_ctx), g(b_ada), g(attn_sink), g(conv_w)
    ln1_g, ln1_b, ln2_g, ln2_b, router_w, router_b = g(ln1_g), g(ln1_b), g(ln2_g), g(ln2_b), g(router_w), g(router_b)
    shared = dict(w_ada=g(w_ada), w_in=g(w_in), w_attn_proj=g(w_attn_proj), w_conv_proj=g(w_conv_proj), w_out=g(w_out))
    shared.update(relayout_experts(g(w_gate), g(w_up), g(w_down)))
    nc = build()
    in_maps = []
    for core in range(8):
        m = prep_core(core, x, c, ctx, c_ctx, b_ada, attn_sink, conv_w, ln1_g, ln1_b, ln2_g, ln2_b, router_w, router_b)
        m.update(shared)
        in_maps.append(m)
    res = run_bass_kernel_spmd(nc, in_maps, core_ids=list(range(8)))
    out = np.empty((2, 16384, D), np.float32)
    for core in range(8):
        y = res.results[core]["yout"]
        b = core // 4
        s = (core % 4) * OWN
        out[b, s:s + OWN, :] = y.reshape(D, OWN).T
    return out
```

```python
import numpy as np
from contextlib import ExitStack
import concourse.bass as bass
import concourse.mybir as mybir
from concourse.bass_utils import run_bass_kernel_spmd

F32 = mybir.dt.float32
BF16 = mybir.dt.bfloat16
AF = mybir.ActivationFunctionType
ALU = mybir.AluOpType
AX = mybir.AxisListType

D = 2048
KC = 16
TL = 4608
TC = 256
TT = TL + TC
OWN = 4096
HALO = 256
INW = 13312
NE = 32
ALPHA = 4.0 ** 0.25
LN_EPS = 1e-5
SCALE = 128.0 ** -0.5
PT = 1792


class Buf:
    __slots__ = ("w", "r", "excl")

    def __init__(self, excl=False):
        self.w = None
        self.r = {}
        self.excl = excl


class Sched:
    ENG = ("pe", "act", "dve", "pool", "sp")

    def __init__(self, nc, stack, ndma=(("sp", 32), ("pool", 32))):
        self.nc = nc
        self.prog = {e: [] for e in self.ENG}
        self.esem = {}
        self.ecnt = {e: 0 for e in self.ENG}
        self.seen = {e: {} for e in self.ENG}
        for e in self.ENG:
            self.esem[e] = stack.enter_context(nc.semaphore(f"es_{e}"))
        self.dring = {}
        self.dpos = {}
        for q, n in ndma:
            self.dring[q] = [[f"d_{q}{i}", stack.enter_context(nc.semaphore(f"d_{q}{i}")), 0] for i in range(n)]
            self.dpos[q] = 0

    def _wait(self, e, tok):
        if tok is None:
            return
        key, sem, val = tok
        if self.seen[e].get(key, 0) >= val:
            return
        self.seen[e][key] = val
        self.prog[e].append(("wait", sem, val))

    def _deps(self, e, reads, writes, pe_chain=False):
        for b in reads:
            self._wait(e, b.w)
        for b in writes:
            if not (pe_chain and b.w is not None and b.w[0] == "es_pe"):
                self._wait(e, b.w)
            for k, (s, v) in b.r.items():
                self._wait(e, (k, s, v))

    def _mark(self, tok, reads, writes):
        key, sem, val = tok
        for b in reads:
            b.r[key] = (sem, val)
        for b in writes:
            b.w = tok
            b.r = {}

    def op(self, e, fn, reads=(), writes=(), pe_chain=False):
        writes = list(writes) + [b for b in reads if b.excl]
        reads = [b for b in reads if not b.excl]
        self._deps(e, reads, writes, pe_chain)
        self.ecnt[e] += 1
        tok = (f"es_{e}", self.esem[e], self.ecnt[e])
        self.prog[e].append(("op", fn, self.esem[e], 1))
        self._mark(tok, reads, writes)
        return tok

    def dma(self, q, fn, reads=(), writes=()):
        ring = self.dring[q]
        slot = ring[self.dpos[q] % len(ring)]
        self.dpos[q] += 1
        key, sem, val = slot
        if val > 0:
            self._wait(q, (key, sem, val))
        self._deps(q, reads, writes)
        slot[2] = val + 16
        tok = (key, sem, val + 16)
        self.prog[q].append(("op", fn, sem, 16))
        self._mark(tok, reads, writes)
        return tok

    def barrier(self):
        toks = []
        for e in self.ENG:
            if self.ecnt[e] > 0:
                toks.append((f"es_{e}", self.esem[e], self.ecnt[e]))
        for q, ring in self.dring.items():
            for key, sem, val in ring:
                if val > 0:
                    toks.append((key, sem, val))
        for e in self.ENG:
            for t in toks:
                self._wait(e, t)

    def emit(self):
        nc = self.nc
        engmap = {"pe": "tensor", "act": "scalar", "dve": "vector", "pool": "gpsimd", "sp": "sync"}
        with nc.Block() as block:
            for e in self.ENG:
                prog = self.prog[e]

                def body(eng, prog=prog):
                    for item in prog:
                        if item[0] == "wait":
                            eng.wait_ge(item[1], item[2])
                        else:
                            item[1](eng).then_inc(item[2], item[3])
                getattr(block, engmap[e])(body)
        self.prog = {e: [] for e in self.ENG}


class Ring:
    def __init__(self, items):
        self.items = items
        self.i = 0

    def next(self):
        it = self.items[self.i % len(self.items)]
        self.i += 1
        return it


def layer_groups(l):
    if l == 0:
        lat_in = [(t, 512, False) for t in range(0, TL, 512)]
        lat = [(t, 512, False) for t in range(128, 4224, 512)] + [(4224, 256, False)]
        ctxg = [(TL, 256, True)]
        return lat_in + ctxg, lat + ctxg
    lat_in = [(t, 512, False) for t in range(128, 4224, 512)] + [(4224, 256, False)]
    lat = [(t, 512, False) for t in range(256, 4352, 512)]
    return lat_in + [(TL, 256, True)], lat


def build(nlayers=2, upto=99, taps=(), small=(), dbg=(), moe="sorted"):
    nc = bass.Bass("TRN2", target_bir_lowering=False)

    def din(name, shape, dt=F32):
        if name in small:
            shape = [1] * len(shape)
        return nc.dram_tensor(name, list(shape), dt, kind="ExternalInput").ap()

    def dscr(name, shape, dt):
        kind = "ExternalOutput" if name in taps else "Internal"
        return nc.dram_tensor(name, list(shape), dt, kind=kind).ap()

    xin = din("xin", [KC, 128, TT])
    cvec = din("cvec", [128, 32])
    badaT = din("badaT", [128, 2 * 96])
    lnp = din("lnp", [128, 2 * 4 * 16])
    cwt_d = din("cwt", [128, 2 * 48])
    sinkrow = din("sinkrow", [2, 2048])
    rw_d = din("rw", [128, 16 * 32])
    rb_d = din("rb", [128, 32])
    cos_d = din("cosT", [128, TL])
    sin_d = din("sinT", [128, TL])
    vbias_d = din("vbias", [128, 36])
    vrow_d = din("vrow", [128, TL])
    consts_d = din("consts", [128, 128 * 2 + 512 * 2 + 32 * 128])
    thr_d = din("thr", [128, 129])
    w_ada = din("w_ada", [2, D, 6 * D])
    w_in = din("w_in", [2, D, INW])
    w_ap = din("w_attn_proj", [2, D, D])
    w_cp = din("w_conv_proj", [2, D, D])
    w_o = din("w_out", [2, D, D])
    NROWS = 2 * NE * 2 * 128
    w_g = din("w_gate", [NROWS * 4, 2048])
    w_u = din("w_up", [NROWS * 4, 2048])
    w_d = din("w_down", [NROWS * 4, 2048])
    yout = nc.dram_tensor("yout", [KC, 128, OWN], F32, kind="ExternalOutput").ap()

    qT = dscr("qT", [16, 128, TT], BF16)
    kT = dscr("kT", [4, 128, TT], BF16)
    vtok = dscr("vtok", [TT, 512], BF16)
    plT = dscr("plT", [80, 128, TT], BF16)
    maT = dscr("maT", [16, 128, TT], BF16)
    mT = dscr("mT", [16, 128, TT], BF16)
    x1T = dscr("x1T", [16, 128, TT], F32)
    h2T = dscr("h2T", [16, 128, TT], BF16)
    fT = dscr("fT", [16, 128, TT], F32)
    x2T = dscr("x2T", [16, 128, TT], F32)
    cwTd = dscr("cwTd", [32, TT], BF16)
    modd = dscr("modd", [2, 128, 192], F32)
    I32 = mybir.dt.int32
    NBMAX = 104
    h2tok = dscr("h2tok", [TT, D], BF16)
    xsd = dscr("xsd", [NBMAX * 128, D], BF16)
    ysd = dscr("ysd", [NBMAX * 128, D], F32)
    slotd = dscr("slotd", [128, 76], I32)
    blked = dscr("blked", [128, 256], I32)
    attd = dscr("attd", [16, 128, TT], BF16)

    with ExitStack() as st:
        S = Sched(nc, st)

        uid = [0]

        def sb(stack, name, shape, dt):
            uid[0] += 1
            return stack.enter_context(nc.sbuf_tensor(f"{name}_u{uid[0]}", list(shape), dt))

        psb = [st.enter_context(nc.psum_tensor(f"ps{i}", [128, 512], F32)) for i in range(8)]
        psB = [Buf(True) for _ in range(8)]
        identb = sb(st, "identb", [128, 128], BF16)
        pswapb = sb(st, "pswapb", [128, 128], BF16)
        triLb = sb(st, "triLb", [128, 512], BF16)
        triUb = sb(st, "triUb", [128, 512], BF16)
        selEb = sb(st, "selEb", [32, 32 * 128], BF16)
        onesb = sb(st, "onesb", [128, 128], BF16)
        onesf = sb(st, "onesf", [128, 128], F32)
        cv = sb(st, "cv", [128, 32], F32)
        scT = sb(st, "scT", [128, 32], F32)
        bada = sb(st, "bada", [128, 192], F32)
        lnt = sb(st, "lnt", [128, 128], F32)
        cwt = sb(st, "cwt_sb", [128, 96], F32)
        rwt = sb(st, "rwt", [128, 512], F32)
        rbt = sb(st, "rbt", [128, 32], F32)
        vbias = sb(st, "vbias_sb", [128, 36], F32)
        modT = [sb(st, f"modT{l}", [128, 192], F32) for l in range(2)]
        modP = [sb(st, f"modP{l}", [128, 192], F32) for l in range(2)]
        cwT = sb(st, "cwT", [32, TT if moe == "dense" else 2], BF16)
        OH = sb(st, "OH", [128, 38 * 64], F32)
        WW = sb(st, "WW", [128, 76], F32)
        POS = sb(st, "POS", [128, 38 * 32], F32)
        SLOT = sb(st, "SLOT", [128, 76], I32)
        BLKE = sb(st, "BLKE", [128, 128], I32)
        CHG = sb(st, "CHG", [128, 128], I32)
        thr = sb(st, "thr_sb", [128, 129], F32)
        IDXT = sb(st, "IDXT", [128, 1024], I32)
        ustr = sb(st, "ustr", [128, 128], BF16)
        brout = Buf()
        bslot = Buf()
        bblk = Buf()
        psbf = [psb[6][:, :].bitcast(BF16), psb[7][:, :].bitcast(BF16)]
        tmpst = ExitStack()
        consts_f = sb(tmpst, "consts_f", [128, 128 * 2 + 512 * 2], F32)
        selEf = sb(tmpst, "selEf", [32, 32 * 128], F32)
        bconst = Buf()
        bmod = [Buf(), Buf()]
        bcwT = Buf()

        CO_ID, CO_PS, CO_TL, CO_TU, CO_SE = 0, 128, 256, 768, 1280
        S.dma("sp", lambda e: e.dma_start(out=consts_f[:, :], in_=consts_d[:, 0:1280]), writes=[bconst])
        S.dma("sp", lambda e: e.dma_start(out=selEf[:, :], in_=consts_d[0:32, 1280:1280 + 4096]), writes=[bconst])
        for (dst, src) in ((cv, cvec), (bada, badaT), (lnt, lnp), (cwt, cwt_d), (rwt, rw_d), (rbt, rb_d), (vbias, vbias_d), (thr, thr_d)):
            S.dma("sp", lambda e, dst=dst, src=src: e.dma_start(out=dst[:, :], in_=src[:, :]), writes=[bconst])
        S.op("dve", lambda e: e.tensor_copy(out=identb[:, :], in_=consts_f[:, CO_ID:CO_ID + 128]), reads=[bconst], writes=[bconst])
        S.op("dve", lambda e: e.tensor_copy(out=pswapb[:, :], in_=consts_f[:, CO_PS:CO_PS + 128]), reads=[bconst], writes=[bconst])
        S.op("dve", lambda e: e.tensor_copy(out=triLb[:, :], in_=consts_f[:, CO_TL:CO_TL + 512]), reads=[bconst], writes=[bconst])
        S.op("dve", lambda e: e.tensor_copy(out=triUb[:, :], in_=consts_f[:, CO_TU:CO_TU + 512]), reads=[bconst], writes=[bconst])
        S.op("dve", lambda e: e.tensor_copy(out=selEb[:, :], in_=selEf[:, :]), reads=[bconst], writes=[bconst])
        S.op("dve", lambda e: e.tensor_tensor(out=ustr[:, :], in0=consts_f[:, CO_TU:CO_TU + 128], in1=consts_f[:, CO_ID:CO_ID + 128], op=ALU.subtract), reads=[bconst], writes=[bconst])
        S.op("pool", lambda e: e.memset(onesb[:, :], 1.0), writes=[bconst])
        S.op("pool", lambda e: e.memset(onesf[:, :], 1.0), writes=[bconst])
        S.op("act", lambda e: e.activation(out=scT[:, :], in_=cv[:, :], func=AF.Silu), reads=[bconst], writes=[bconst])
        S.barrier()

        def phase_mod(l):
            with ExitStack() as ph:
                wst = Ring([(sb(ph, f"wada{i}", [128, 16 * 512], F32), Buf()) for i in range(2)])
                for jb in range(24):
                    wt, bw = wst.next()
                    S.dma("sp", lambda e, wt=wt, jb=jb: e.dma_start(
                        out=wt[:, :].rearrange("p (k c) -> p k c", k=16),
                        in_=w_ada[l, :, jb * 512:(jb + 1) * 512].rearrange("(k p) c -> p k c", p=128)), writes=[bw])
                    for cc in range(4):
                        j = jb * 4 + cc
                        pi = j % 2
                        for k in range(16):
                            S.op("pe", lambda e, wt=wt, k=k, cc=cc, pi=pi: e.matmul(
                                psb[pi][:, 0:2], wt[:, k * 512 + cc * 128:k * 512 + (cc + 1) * 128], scT[:, 2 * k:2 * k + 2],
                                start=(k == 0), stop=(k == 15)), reads=[bw, bconst], writes=[psB[pi]], pe_chain=(k > 0))
                        S.op("dve", lambda e, j=j, pi=pi: e.tensor_scalar(
                            out=modT[l][:, 2 * j:2 * j + 2], in0=psb[pi][:, 0:2], scalar1=bada[:, l * 96 + j:l * 96 + j + 1], scalar2=None,
                            op0=ALU.add), reads=[psB[pi], bconst], writes=[bmod[l]])
                S.op("dve", lambda e: e.tensor_scalar(out=modP[l][:, :], in0=modT[l][:, :], scalar1=1.0, scalar2=None, op0=ALU.add),
                     reads=[bmod[l]], writes=[bmod[l]])
                if "modd" in taps:
                    S.dma("sp", lambda e: e.dma_start(out=modd[l], in_=modT[l][:, :]), reads=[bmod[l]])
                S.barrier()
                S.emit()

        def mcol(l, i, k, s, plus=False):
            t = modP[l] if plus else modT[l]
            c = 2 * (i * 16 + k) + s
            return t[:, c:c + 1]

        def phase_inproj(l, xres):
            groups_in, _ = layer_groups(l)
            passes = []
            cur = []
            tot = 0
            for g in groups_in:
                if tot + g[1] > PT:
                    passes.append(cur)
                    cur, tot = [], 0
                cur.append(g)
                tot += g[1]
            passes.append(cur)
            with ExitStack() as ph:
                hT = sb(ph, "hT", [128, 16 * PT], BF16)
                bh = Buf()
                xst = Ring([(sb(ph, f"xst{i}", [128, 16 * 512], F32), Buf()) for i in range(1)])
                wbr = Ring([(sb(ph, f"wb{i}", [128, 16 * 256], BF16), Buf()) for i in range(3)])
                cost = sb(ph, "cost", [128, PT], F32)
                sint = sb(ph, "sint", [128, PT], F32)
                btab = Buf()
                qbr = Ring([(sb(ph, f"qb{i}", [128, 512], BF16), Buf()) for i in range(2)])
                t1r = Ring([(sb(ph, f"t1_{i}", [128, 512], F32), Buf()) for i in range(2)])
                t2r = Ring([(sb(ph, f"t2_{i}", [128, 512], F32), Buf()) for i in range(2)])
                obr = Ring([(sb(ph, f"ob{i}", [128, 512], BF16), Buf()) for i in range(4)])
                psr = Ring([(psb[i], psB[i]) for i in range(4)])
                ps2r = Ring([(psb[i], psB[i]) for i in range(4, 6)])
                evi = [0]
                for pgroups in passes:
                    offs = []
                    off = 0
                    for (t0, n, isc) in pgroups:
                        offs.append(off)
                        off += n
                    for (t0, n, isc), off in zip(pgroups, offs):
                        xt, bx = xst.next()
                        S.dma("sp", lambda e, xt=xt, t0=t0, n=n: e.dma_start(
                            out=xt[:, 0:16 * n].rearrange("p (k t) -> p k t", k=16),
                            in_=xres[:, :, t0:t0 + n].rearrange("k p t -> p k t")), writes=[bx])
                        s = 1 if isc else 0
                        for k in range(16):
                            S.op("act", lambda e, xt=xt, k=k, n=n, off=off, s=s: e.activation(
                                out=hT[:, k * PT + off:k * PT + off + n], in_=xt[:, k * n:(k + 1) * n], func=AF.Identity,
                                bias=mcol(l, 0, k, s), scale=mcol(l, 1, k, s, True)), reads=[bx, bmod[l]], writes=[bh])
                        if not isc:
                            S.dma("sp", lambda e, t0=t0, n=n, off=off: e.dma_start(out=cost[:, off:off + n], in_=cos_d[:, t0:t0 + n]), writes=[btab])
                            S.dma("sp", lambda e, t0=t0, n=n, off=off: e.dma_start(out=sint[:, off:off + n], in_=sin_d[:, t0:t0 + n]), writes=[btab])
                    for cb in range(INW // 256):
                        c0 = cb * 256
                        isv = 2560 <= c0 < 3072
                        iskv = 2048 <= c0 < 3072
                        if l == 1 and all(g[2] for g in pgroups) and not iskv:
                            continue
                        if 'cb4' in dbg and cb >= 4 and not (cb in (10, 11) and 'v' in dbg):
                            continue
                        wb, bw = wbr.next()
                        S.dma("pool", lambda e, wb=wb, c0=c0: e.dma_start(
                            out=wb[:, :].rearrange("p (k c) -> p k c", k=16),
                            in_=w_in[l, :, c0:c0 + 256].rearrange("(k p) c -> p k c", p=128)), writes=[bw])
                        if isv and 'nov' in dbg:
                            continue
                        if isv:
                            for (t0, n, isc), off in zip(pgroups, offs):
                                for ti in range(n // 128):
                                    ps, bp = psr.next()
                                    for k in range(16):
                                        S.op("pe", lambda e, ps=ps, wb=wb, k=k, o=off + ti * 128: e.matmul(
                                            ps[:, 0:256], hT[:, k * PT + o:k * PT + o + 128], wb[:, k * 256:(k + 1) * 256],
                                            start=(k == 0), stop=(k == 15)), reads=[bw, bh], writes=[bp], pe_chain=(k > 0))
                                    ob, bo = obr.next()
                                    S.op("act", lambda e, ob=ob, ps=ps: e.activation(out=ob[:, 0:256], in_=ps[:, 0:256], func=AF.Copy),
                                         reads=[bp], writes=[bo])
                                    r0 = t0 + ti * 128
                                    S.dma("sp", lambda e, ob=ob, r0=r0, c0=c0: e.dma_start(
                                        out=vtok[r0:r0 + 128, c0 - 2560:c0 - 2560 + 256], in_=ob[:, 0:256]), reads=[bo])
                            continue
                        for cc in range(2):
                            gc = cb * 2 + cc
                            for (t0, n, isc), off in zip(pgroups, offs):
                                if l == 1 and isc and not iskv:
                                    continue
                                ps, bp = psr.next()
                                for k in range(16):
                                    S.op("pe", lambda e, ps=ps, wb=wb, k=k, cc=cc, off=off, n=n: e.matmul(
                                        ps[:, 0:n], wb[:, k * 256 + cc * 128:k * 256 + (cc + 1) * 128], hT[:, k * PT + off:k * PT + off + n],
                                        start=(k == 0), stop=(k == 15)), reads=[bw, bh], writes=[bp], pe_chain=(k > 0))
                                if gc < 16:
                                    dst = qT[gc]
                                elif gc < 20:
                                    dst = kT[gc - 16]
                                else:
                                    dst = plT[gc - 24]
                                ob, bo = obr.next()
                                if gc < 20 and not isc and 'norope' not in dbg:
                                    qb, bq = qbr.next()
                                    S.op("act", lambda e, qb=qb, ps=ps, n=n: e.activation(out=qb[:, 0:n], in_=ps[:, 0:n], func=AF.Copy),
                                         reads=[bp], writes=[bq])
                                    ps2, bp2 = ps2r.next()
                                    S.op("pe", lambda e, ps2=ps2, qb=qb, n=n: e.matmul(ps2[:, 0:n], pswapb[:, :], qb[:, 0:n], start=True, stop=True),
                                         reads=[bq, bconst], writes=[bp2])
                                    t1, b1 = t1r.next()
                                    t2, b2 = t2r.next()
                                    S.op("dve", lambda e, t1=t1, ps=ps, n=n, off=off: e.tensor_tensor(
                                        out=t1[:, 0:n], in0=ps[:, 0:n], in1=cost[:, off:off + n], op=ALU.mult), reads=[bp, btab], writes=[b1])
                                    S.op("dve", lambda e, t2=t2, ps2=ps2, n=n, off=off: e.tensor_tensor(
                                        out=t2[:, 0:n], in0=ps2[:, 0:n], in1=sint[:, off:off + n], op=ALU.mult), reads=[bp2, btab], writes=[b2])
                                    S.op("dve" if "ropedve" in dbg else "pool", lambda e, ob=ob, t1=t1, t2=t2, n=n: e.tensor_tensor(
                                        out=ob[:, 0:n], in0=t1[:, 0:n], in1=t2[:, 0:n], op=ALU.add), reads=[b1, b2], writes=[bo])
                                else:
                                    evi[0] += 1
                                    if evi[0] % 2 == 0:
                                        S.op("act", lambda e, ob=ob, ps=ps, n=n: e.activation(out=ob[:, 0:n], in_=ps[:, 0:n], func=AF.Copy),
                                             reads=[bp], writes=[bo])
                                    else:
                                        S.op("dve", lambda e, ob=ob, ps=ps, n=n: e.tensor_copy(out=ob[:, 0:n], in_=ps[:, 0:n]),
                                             reads=[bp], writes=[bo])
                                S.dma("sp", lambda e, ob=ob, dst=dst, t0=t0, n=n: e.dma_start(out=dst[:, t0:t0 + n], in_=ob[:, 0:n]), reads=[bo])
                S.barrier()
                S.emit()

        def load_w2048(ph, name, wd, l):
            wt = sb(ph, name, [128, 16 * 2048], BF16)
            bws = [Buf() for _ in range(4)]
            for c in range(4):
                S.dma("pool", lambda e, c=c: e.dma_start(
                    out=wt[:, :].rearrange("p (k c) -> p k c", k=16)[:, :, c * 512:(c + 1) * 512],
                    in_=wd[l, :, c * 512:(c + 1) * 512].rearrange("(k p) c -> p k c", p=128)), writes=[bws[c]])
            return wt, bws

        def phase_attn(l):
            _, groups = layer_groups(l)
            with ExitStack() as ph:
                wa, bwa = load_w2048(ph, "wa", w_ap, l)
                kcT = sb(ph, "kcT", [128, 4 * 256], BF16)
                vcs = sb(ph, "vcs", [128, 2 * 512], BF16)
                bkc = Buf()
                S.dma("sp", lambda e: e.dma_start(out=kcT[:, :].rearrange("p (h t) -> p h t", h=4),
                                                  in_=kT[:, :, TL:TL + 256].rearrange("h p t -> p h t")), writes=[bkc])
                S.dma("sp", lambda e: e.dma_start(out=vcs[:, :].rearrange("p (j c) -> p j c", j=2),
                                                  in_=vtok[TL:TL + 256, :].rearrange("(j p) c -> p j c", p=128)), writes=[bkc])
                sk_f = sb(ph, "sk_f", [1, 2048], F32)
                esink = sb(ph, "esink", [1, 2048], BF16)
                bsk = Buf()
                S.dma("sp", lambda e: e.dma_start(out=sk_f[:, :], in_=sinkrow[l:l + 1, :]), writes=[bsk])
                S.op("act", lambda e: e.activation(out=esink[:, :], in_=sk_f[:, :], func=AF.Exp), reads=[bsk], writes=[bsk])
                qsr = Ring([(sb(ph, f"qs{i}", [128, 16 * 512], BF16), Buf()) for i in range(1)])
                ksr = Ring([(sb(ph, f"ks{i}", [128, 4 * 768], BF16), Buf()) for i in range(2)])
                vsr = Ring([(sb(ph, f"vs{i}", [128, 6 * 512], BF16), Buf()) for i in range(2)])
                atr = Ring([(sb(ph, f"at{i}", [128, 16 * 512], BF16), Buf()) for i in range(1)])
                ptr = Ring([(sb(ph, f"pt{i}", [128, 512], BF16), Buf()) for i in range(4)])
                rdr = Ring([(sb(ph, f"rd{i}", [128, 512], F32), Buf()) for i in range(2)])
                gar = Ring([(sb(ph, f"ga{i}", [128, 512], BF16), Buf()) for i in range(4)])
                sgr = Ring([(sb(ph, f"sg{i}", [128, 512], F32), Buf()) for i in range(4)])
                mar = Ring([(sb(ph, f"mao{i}", [128, 512], BF16), Buf()) for i in range(4)])
                pss = Ring([(psb[i], psB[i]) for i in (0, 1)])
                pso = Ring([(psb[i], psB[i]) for i in (2, 3)])
                psd = Ring([(psb[i], psB[i]) for i in (4, 5)])
                psa = Ring([(psb[i], psB[i]) for i in (6, 7)])
                for (t0, n, isc) in groups:
                    nt = n // 128
                    qs, bqs = qsr.next()
                    S.dma("sp", lambda e, qs=qs, t0=t0, n=n: e.dma_start(
                        out=qs[:, 0:16 * n].rearrange("p (h t) -> p h t", h=16), in_=qT[:, :, t0:t0 + n].rearrange("h p t -> p h t")), writes=[bqs])
                    if not isc:
                        kw = n + 256
                        ks, bks = ksr.next()
                        vs, bvs = vsr.next()
                        S.dma("sp", lambda e, ks=ks, t0=t0, kw=kw: e.dma_start(
                            out=ks[:, 0:4 * kw].rearrange("p (h t) -> p h t", h=4),
                            in_=kT[:, :, t0 - 128:t0 - 128 + kw].rearrange("h p t -> p h t")), writes=[bks])
                        S.dma("sp", lambda e, vs=vs, t0=t0, kw=kw: e.dma_start(
                            out=vs[:, 0:(kw // 128) * 512].rearrange("p (j c) -> p j c", c=512),
                            in_=vtok[t0 - 128:t0 - 128 + kw, :].rearrange("(j p) c -> p j c", p=128)), writes=[bvs])
                    at, bat = atr.next()
                    for i in range(nt):
                        for h in range(4):
                            kts = []
                            if not isc:
                                for kk in range(3):
                                    gt = t0 // 128 - 1 + i + kk
                                    kts.append((ks[:, h * kw + (i + kk) * 128:h * kw + (i + kk + 1) * 128],
                                                vs[:, (i + kk) * 512 + h * 128:(i + kk) * 512 + (h + 1) * 128],
                                                vbias[:, gt:gt + 1], (triLb if kk == 0 else (triUb if kk == 2 else None)), [bks, bvs]))
                            for j in range(2):
                                kts.append((kcT[:, h * 256 + j * 128:h * 256 + (j + 1) * 128],
                                            vcs[:, j * 512 + h * 128:j * 512 + (h + 1) * 128], 0.0, None, [bkc]))
                            po, bpo = pso.next()
                            pd, bpd = psd.next()
                            qv = qs[:, 0:16 * n].rearrange("p (h t) -> p h t", h=16)[:, 4 * h:4 * h + 4, i * 128:(i + 1) * 128]
                            nk = len(kts)
                            pts = [None] * nk
                            for ki in range(nk + 1):
                                if ki < nk:
                                    kap, vap, bias, msk, deps = kts[ki]
                                    p_s, bps_ = pss.next()
                                    S.op("pe", lambda e, p_s=p_s, kap=kap, qv=qv: e.matmul(
                                        p_s[:, 0:512].rearrange("p (g q) -> p g q", g=4), kap, qv, start=True, stop=True),
                                        reads=deps + [bqs], writes=[bps_])
                                    pt, bpt = ptr.next()
                                    pts[ki] = (pt, bpt)
                                    S.op("act", lambda e, pt=pt, p_s=p_s, bias=bias: e.activation(
                                        out=pt[:, :], in_=p_s[:, 0:512], func=AF.Exp, bias=bias, scale=SCALE), reads=[bps_, bconst], writes=[bpt])
                                    if msk is not None:
                                        S.op("pool", lambda e, pt=pt, msk=msk: e.tensor_tensor(out=pt[:, :], in0=pt[:, :], in1=msk[:, :], op=ALU.mult),
                                             reads=[bpt, bconst], writes=[bpt])
                                if ki >= 1:
                                    kj = ki - 1
                                    kap, vap, bias, msk, deps = kts[kj]
                                    pt, bpt = pts[kj]
                                    S.op("pe", lambda e, po=po, vap=vap, pt=pt, kj=kj, nk=nk: e.matmul(po[:, 0:512], vap, pt[:, :], start=(kj == 0), stop=(kj == nk - 1)),
                                         reads=deps + [bpt], writes=[bpo], pe_chain=(kj > 0))
                                    S.op("pe", lambda e, pd=pd, pt=pt, kj=kj: e.matmul(pd[:, 0:512], onesb[:, :], pt[:, :], start=(kj == 0), stop=False),
                                         reads=[bpt, bconst], writes=[bpd], pe_chain=(kj > 0))
                            S.op("pe", lambda e, pd=pd, h=h: e.matmul(pd[:, 0:512], onesb[0:1, :], esink[0:1, h * 512:(h + 1) * 512], start=False, stop=True),
                                 reads=[bsk, bconst], writes=[bpd], pe_chain=True)
                            rd, brd = rdr.next()
                            S.op("dve", lambda e, rd=rd, pd=pd: e.reciprocal(out=rd[:, :], in_=pd[:, 0:512]), reads=[bpd], writes=[brd])
                            av = at[:, 0:16 * n].rearrange("p (h t) -> p h t", h=16)[:, 4 * h:4 * h + 4, i * 128:(i + 1) * 128]
                            S.op("dve", lambda e, av=av, po=po, rd=rd: e.tensor_tensor(
                                out=av, in0=po[:, 0:512].rearrange("p (g q) -> p g q", g=4), in1=rd[:, :].rearrange("p (g q) -> p g q", g=4), op=ALU.mult),
                                reads=[bpo, brd], writes=[bat])
                    if "attd" in taps:
                        S.dma("sp", lambda e, at=at, t0=t0, n=n: e.dma_start(out=attd[:, :, t0:t0 + n].rearrange("h p t -> p h t"),
                                                                            in_=at[:, 0:16 * n].rearrange("p (h t) -> p h t", h=16)), reads=[bat])
                    for c in range(16):
                        pa, bpa = psa.next()
                        for k in range(16):
                            S.op("pe", lambda e, pa=pa, k=k, c=c, at=at, n=n: e.matmul(
                                pa[:, 0:n], wa[:, k * 2048 + c * 128:k * 2048 + (c + 1) * 128], at[:, k * n:(k + 1) * n],
                                start=(k == 0), stop=(k == 15)), reads=[bwa[c // 4], bat], writes=[bpa], pe_chain=(k > 0))
                        ga, bga = gar.next()
                        S.dma("sp", lambda e, ga=ga, c=c, t0=t0, n=n: e.dma_start(out=ga[:, 0:n], in_=plT[48 + c][:, t0:t0 + n]), writes=[bga])
                        sg, bsg = sgr.next()
                        S.op("act", lambda e, sg=sg, ga=ga, n=n: e.activation(out=sg[:, 0:n], in_=ga[:, 0:n], func=AF.Sigmoid), reads=[bga], writes=[bsg])
                        mo, bmo = mar.next()
                        S.op("dve", lambda e, mo=mo, pa=pa, sg=sg, n=n: e.tensor_tensor(out=mo[:, 0:n], in0=pa[:, 0:n], in1=sg[:, 0:n], op=ALU.mult),
                             reads=[bpa, bsg], writes=[bmo])
                        S.dma("sp", lambda e, mo=mo, c=c, t0=t0, n=n: e.dma_start(out=maT[c][:, t0:t0 + n], in_=mo[:, 0:n]), reads=[bmo])
                S.barrier()
                S.emit()

        def phase_conv(l):
            _, groups = layer_groups(l)
            with ExitStack() as ph:
                ws, bws = load_w2048(ph, "ws", w_cp, l)
                vrow = sb(ph, "vrow", [128, TL], F32)
                bvr = Buf()
                S.dma("sp", lambda e: e.dma_start(out=vrow[:, :], in_=vrow_d[:, :]), writes=[bvr])
                zr = Ring([(sb(ph, f"z{i}", [128, 16 * 512], BF16), Buf()) for i in range(2)])
                cbr = Ring([(sb(ph, f"cb{i}", [128, 512], BF16), Buf()) for i in range(4)])
                ccr = Ring([(sb(ph, f"cc{i}", [128, 514], BF16), Buf()) for i in range(4)])
                cur = Ring([(sb(ph, f"cu{i}", [128, 514], BF16), Buf()) for i in range(4)])
                ur = Ring([(sb(ph, f"u{i}", [128, 514], F32), Buf()) for i in range(3)])
                tr = Ring([(sb(ph, f"t{i}", [128, 512], F32), Buf()) for i in range(3)])
                gcr = Ring([(sb(ph, f"gc{i}", [128, 512], BF16), Buf()) for i in range(4)])
                mair = Ring([(sb(ph, f"mai{i}", [128, 512], BF16), Buf()) for i in range(4)])
                sgr = Ring([(sb(ph, f"sg{i}", [128, 512], F32), Buf()) for i in range(2)])
                t3r = Ring([(sb(ph, f"t3_{i}", [128, 512], F32), Buf()) for i in range(2)])
                mor = Ring([(sb(ph, f"mo{i}", [128, 512], BF16), Buf()) for i in range(4)])
                psr = Ring([(psb[i], psB[i]) for i in range(4)])
                for (t0, n, isc) in groups:
                    z, bz = zr.next()
                    for k in range(16):
                        cbt, bcb = cbr.next()
                        cct, bcc = ccr.next()
                        cut, bcu = cur.next()
                        S.dma("sp", lambda e, cbt=cbt, k=k, t0=t0, n=n: e.dma_start(out=cbt[:, 0:n], in_=plT[k][:, t0:t0 + n]), writes=[bcb])
                        if isc:
                            S.op("pool", lambda e, cct=cct, n=n: e.memset(cct[:, 0:n + 2], 0.0), writes=[bcc])
                            S.dma("sp", lambda e, cct=cct, k=k, t0=t0, n=n: e.dma_start(out=cct[:, 1:n + 1], in_=plT[16 + k][:, t0:t0 + n]), writes=[bcc])
                            S.dma("sp", lambda e, cut=cut, k=k, t0=t0, n=n: e.dma_start(out=cut[:, 1:n + 1], in_=plT[32 + k][:, t0:t0 + n]), writes=[bcu])
                            S.op("pool", lambda e, cut=cut, n=n: e.memset(cut[:, 0:1], 0.0), reads=[bcu], writes=[bcu])
                            S.op("pool", lambda e, cut=cut, n=n: e.memset(cut[:, n + 1:n + 2], 0.0), reads=[bcu], writes=[bcu])
                        else:
                            S.dma("sp", lambda e, cct=cct, k=k, t0=t0, n=n: e.dma_start(out=cct[:, 0:n + 2], in_=plT[16 + k][:, t0 - 1:t0 + n + 1]), writes=[bcc])
                            S.dma("sp", lambda e, cut=cut, k=k, t0=t0, n=n: e.dma_start(out=cut[:, 0:n + 2], in_=plT[32 + k][:, t0 - 1:t0 + n + 1]), writes=[bcu])
                        u, bu = ur.next()
                        S.op("pool", lambda e, u=u, cct=cct, cut=cut, n=n: e.tensor_tensor(out=u[:, 0:n + 2], in0=cct[:, 0:n + 2], in1=cut[:, 0:n + 2], op=ALU.mult),
                             reads=[bcc, bcu], writes=[bu])
                        if not isc:
                            S.op("pool", lambda e, u=u, t0=t0, n=n: e.tensor_tensor(out=u[:, 0:n + 2], in0=u[:, 0:n + 2], in1=vrow[:, t0 - 1:t0 + n + 1], op=ALU.mult),
                                 reads=[bu, bvr], writes=[bu])
                        t, bt = tr.next()
                        wc = lambda j, k=k: cwt[:, l * 48 + k * 3 + j:l * 48 + k * 3 + j + 1]
                        S.op("dve", lambda e, t=t, u=u, n=n, wc=wc: e.tensor_scalar(out=t[:, 0:n], in0=u[:, 0:n], scalar1=wc(0), scalar2=None, op0=ALU.mult),
                             reads=[bu, bconst], writes=[bt])
                        S.op("dve", lambda e, t=t, u=u, n=n, wc=wc: e.scalar_tensor_tensor(out=t[:, 0:n], in0=u[:, 1:n + 1], scalar=wc(1), in1=t[:, 0:n], op0=ALU.mult, op1=ALU.add),
                             reads=[bu, bconst, bt], writes=[bt])
                        S.op("dve", lambda e, t=t, u=u, n=n, wc=wc: e.scalar_tensor_tensor(out=t[:, 0:n], in0=u[:, 2:n + 2], scalar=wc(2), in1=t[:, 0:n], op0=ALU.mult, op1=ALU.add),
                             reads=[bu, bconst, bt], writes=[bt])
                        S.op("pool", lambda e, z=z, k=k, n=n, cbt=cbt, t=t: e.tensor_tensor(out=z[:, k * n:(k + 1) * n], in0=cbt[:, 0:n], in1=t[:, 0:n], op=ALU.mult),
                             reads=[bcb, bt], writes=[bz])
                    for c in range(16):
                        ps, bp = psr.next()
                        for k in range(16):
                            S.op("pe", lambda e, ps=ps, k=k, c=c, z=z, n=n: e.matmul(
                                ps[:, 0:n], ws[:, k * 2048 + c * 128:k * 2048 + (c + 1) * 128], z[:, k * n:(k + 1) * n],
                                start=(k == 0), stop=(k == 15)), reads=[bws[c // 4], bz], writes=[bp], pe_chain=(k > 0))
                        gct, bgc = gcr.next()
                        mai, bmai = mair.next()
                        S.dma("sp", lambda e, gct=gct, c=c, t0=t0, n=n: e.dma_start(out=gct[:, 0:n], in_=plT[64 + c][:, t0:t0 + n]), writes=[bgc])
                        S.dma("sp", lambda e, mai=mai, c=c, t0=t0, n=n: e.dma_start(out=mai[:, 0:n], in_=maT[c][:, t0:t0 + n]), writes=[bmai])
                        sg, bsg = sgr.next()
                        S.op("act", lambda e, sg=sg, gct=gct, n=n: e.activation(out=sg[:, 0:n], in_=gct[:, 0:n], func=AF.Sigmoid), reads=[bgc], writes=[bsg])
                        t3, bt3 = t3r.next()
                        S.op("dve", lambda e, t3=t3, ps=ps, sg=sg, n=n: e.tensor_tensor(out=t3[:, 0:n], in0=ps[:, 0:n], in1=sg[:, 0:n], op=ALU.mult),
                             reads=[bp, bsg], writes=[bt3])
                        mo, bmo = mor.next()
                        S.op("pool", lambda e, mo=mo, t3=t3, mai=mai, n=n: e.tensor_tensor(out=mo[:, 0:n], in0=t3[:, 0:n], in1=mai[:, 0:n], op=ALU.add),
                             reads=[bt3, bmai], writes=[bmo])
                        S.dma("sp", lambda e, mo=mo, c=c, t0=t0, n=n: e.dma_start(out=mT[c][:, t0:t0 + n], in_=mo[:, 0:n]), reads=[bmo])
                S.barrier()
                S.emit()

        def layer_norm_fm(ph_tiles, z, bz, n, gcol, bcol):
            (sqr, mean, m2, rstd, bst, pssum, pssq) = ph_tiles
            (ps1, bp1), (ps2, bp2) = pssum, pssq
            for c in range(16):
                S.op("pe", lambda e, c=c: e.matmul(ps1[:, 0:n], onesf[:, :], z[:, c * n:(c + 1) * n], start=(c == 0), stop=(c == 15)),
                     reads=[bz, bconst], writes=[bp1], pe_chain=(c > 0))
                sq, bsq = sqr.next()
                S.op("act", lambda e, sq=sq, c=c: e.activation(out=sq[:, 0:n], in_=z[:, c * n:(c + 1) * n], func=AF.Square), reads=[bz], writes=[bsq])
                S.op("pe", lambda e, sq=sq, c=c: e.matmul(ps2[:, 0:n], onesf[:, :], sq[:, 0:n], start=(c == 0), stop=(c == 15)),
                     reads=[bsq, bconst], writes=[bp2], pe_chain=(c > 0))
            S.op("dve", lambda e: e.tensor_scalar(out=mean[:, 0:n], in0=ps1[:, 0:n], scalar1=1.0 / D, scalar2=None, op0=ALU.mult), reads=[bp1], writes=[bst])
            S.op("pool", lambda e: e.tensor_tensor(out=m2[:, 0:n], in0=mean[:, 0:n], in1=mean[:, 0:n], op=ALU.mult), reads=[bst], writes=[bst])
            S.op("dve", lambda e: e.scalar_tensor_tensor(out=rstd[:, 0:n], in0=ps2[:, 0:n], scalar=1.0 / D, in1=m2[:, 0:n], op0=ALU.mult, op1=ALU.subtract),
                 reads=[bp2, bst], writes=[bst])
            S.op("dve", lambda e: e.tensor_scalar(out=rstd[:, 0:n], in0=rstd[:, 0:n], scalar1=LN_EPS, scalar2=None, op0=ALU.add),
                 reads=[bst], writes=[bst])
            S.op("act", lambda e: e.activation(out=rstd[:, 0:n], in_=rstd[:, 0:n], func=AF.Sqrt), reads=[bst], writes=[bst])
            S.op("dve", lambda e: e.reciprocal(out=rstd[:, 0:n], in_=rstd[:, 0:n]), reads=[bst], writes=[bst])
            for c in range(16):
                zc = z[:, c * n:(c + 1) * n]
                S.op("dve", lambda e, zc=zc: e.tensor_tensor(out=zc, in0=zc, in1=mean[:, 0:n], op=ALU.subtract), reads=[bz, bst], writes=[bz])
                S.op("pool", lambda e, zc=zc: e.tensor_tensor(out=zc, in0=zc, in1=rstd[:, 0:n], op=ALU.mult), reads=[bz, bst], writes=[bz])
                S.op("act", lambda e, zc=zc, c=c: e.activation(out=zc, in_=zc, func=AF.Identity, bias=bcol(c), scale=gcol(c)), reads=[bz, bconst], writes=[bz])

        def ln_tiles(ph):
            return (Ring([(sb(ph, f"sq{i}", [128, 512], F32), Buf()) for i in range(2)]),
                    sb(ph, "mean", [128, 512], F32), sb(ph, "m2", [128, 512], F32), sb(ph, "rstd", [128, 512], F32), Buf(),
                    (psb[4], psB[4]), (psb[5], psB[5]))

        def phase_out(l, xres):
            _, groups = layer_groups(l)
            with ExitStack() as ph:
                wo, bwo = load_w2048(ph, "wo", w_o, l)
                lnt_t = ln_tiles(ph)
                msr = Ring([(sb(ph, f"ms{i}", [128, 16 * 512], BF16), Buf()) for i in range(1)])
                xsr = Ring([(sb(ph, f"xs{i}", [128, 16 * 512], F32), Buf()) for i in range(1)])
                h2b = sb(ph, "h2b", [128, 16 * 512], BF16)
                bh2b = Buf()
                psr = Ring([(psb[i], psB[i]) for i in range(3)])
                h2t = sb(ph, "h2t", [128, 2048], BF16)
                bh2t = Buf()
                R = {nm: (sb(ph, "r_" + nm, [128, w], F32), Buf()) for nm, w in
                     (("lg", 32), ("ex", 32), ("pr", 32), ("sel", 32), ("eq", 32), ("sel2", 32), ("oh1", 32), ("oh2", 32), ("tmp", 32),
                      ("m1g", 4), ("m2g", 4), ("gs", 4), ("gh", 4), ("t4", 4), ("sc", 16))}
                cwb = sb(ph, "cwb", [128, 32], BF16)
                bcwb = Buf()
                brt = Buf()
                for (t0, n, isc) in groups:
                    s = 1 if isc else 0
                    ms, bms = msr.next()
                    xs, bxs = xsr.next()
                    S.dma("sp", lambda e, ms=ms, t0=t0, n=n: e.dma_start(out=ms[:, 0:16 * n].rearrange("p (k t) -> p k t", k=16),
                                                                        in_=mT[:, :, t0:t0 + n].rearrange("k p t -> p k t")), writes=[bms])
                    S.dma("sp", lambda e, xs=xs, t0=t0, n=n: e.dma_start(out=xs[:, 0:16 * n].rearrange("p (k t) -> p k t", k=16),
                                                                        in_=xres[:, :, t0:t0 + n].rearrange("k p t -> p k t")), writes=[bxs])
                    for c in range(16):
                        ps, bp = psr.next()
                        for k in range(16):
                            S.op("pe", lambda e, ps=ps, k=k, c=c, ms=ms, n=n: e.matmul(
                                ps[:, 0:n], wo[:, k * 2048 + c * 128:k * 2048 + (c + 1) * 128], ms[:, k * n:(k + 1) * n],
                                start=(k == 0), stop=(k == 15)), reads=[bwo[c // 4], bms], writes=[bp], pe_chain=(k > 0))
                        xc = xs[:, c * n:(c + 1) * n]
                        S.op("act", lambda e, xc=xc: e.activation(out=xc, in_=xc, func=AF.Copy, scale=ALPHA), reads=[bxs], writes=[bxs])
                        S.op("dve", lambda e, xc=xc, ps=ps, c=c, n=n, s=s: e.scalar_tensor_tensor(
                            out=xc, in0=ps[:, 0:n], scalar=mcol(l, 2, c, s), in1=xc, op0=ALU.mult, op1=ALU.add), reads=[bp, bxs, bmod[l]], writes=[bxs])
                    if "noln" not in dbg:
                        layer_norm_fm(lnt_t, xs, bxs, n,
                                      lambda c: lnt[:, l * 64 + c:l * 64 + c + 1], lambda c: lnt[:, l * 64 + 16 + c:l * 64 + 16 + c + 1])
                    S.dma("sp", lambda e, xs=xs, t0=t0, n=n: e.dma_start(out=x1T[:, :, t0:t0 + n].rearrange("k p t -> p k t"),
                                                                        in_=xs[:, 0:16 * n].rearrange("p (k t) -> p k t", k=16)), reads=[bxs])
                    h2f, bh2f = xs, bxs
                    for c in range(16):
                        S.op("act", lambda e, xs=xs, c=c, n=n, s=s: e.activation(
                            out=xs[:, c * n:(c + 1) * n], in_=xs[:, c * n:(c + 1) * n], func=AF.Identity,
                            bias=mcol(l, 3, c, s), scale=mcol(l, 4, c, s, True)), reads=[bxs, bmod[l]], writes=[bxs])
                        S.op("pool", lambda e, xs=xs, c=c, n=n: e.tensor_copy(out=h2b[:, c * n:(c + 1) * n], in_=xs[:, c * n:(c + 1) * n]), reads=[bxs], writes=[bh2b])
                    S.dma("sp", lambda e, t0=t0, n=n: e.dma_start(out=h2T[:, :, t0:t0 + n].rearrange("k p t -> p k t"),
                                                                 in_=h2b[:, 0:16 * n].rearrange("p (k t) -> p k t", k=16)), reads=[bh2b])
                    if moe == "sorted":
                        for i in range(n // 128):
                            for c in range(16):
                                S.op("pe", lambda e, c=c, i=i, n=n: e.transpose(psbf[c // 8][:, (c % 8) * 128:(c % 8 + 1) * 128],
                                                                                h2b[:, c * n + i * 128:c * n + (i + 1) * 128], identb[:, :]),
                                     reads=[bh2b, bconst], writes=[psB[6 + c // 8]])
                            S.op("act", lambda e: e.activation(out=h2t[:, 0:1024], in_=psbf[0][:, :], func=AF.Copy), reads=[psB[6]], writes=[bh2t])
                            S.op("dve", lambda e: e.tensor_copy(out=h2t[:, 1024:2048], in_=psbf[1][:, :]), reads=[psB[7]], writes=[bh2t])
                            r0 = t0 + i * 128
                            S.dma("sp", lambda e, r0=r0: e.dma_start(out=h2tok[r0:r0 + 128, :], in_=h2t[:, :]), reads=[bh2t])
                    for i in range(0 if 'norouter' in dbg else n // 128):
                        pl, bpl = psb[3], psB[3]
                        for k in range(16):
                            S.op("pe", lambda e, k=k, i=i, n=n, h2f=h2f: e.matmul(pl[:, 0:32], h2f[:, k * n + i * 128:k * n + (i + 1) * 128], rwt[:, k * 32:(k + 1) * 32],
                                                                         start=(k == 0), stop=(k == 15)), reads=[bh2f, bconst], writes=[bpl], pe_chain=(k > 0))
                        T = {k_: v[0] for k_, v in R.items()}
                        sc = T["sc"]

                        rstop = [int(x[5:]) for x in dbg if x.startswith("rstop")]
                        rstop = rstop[0] if rstop else 10 ** 6
                        vcnt = [0]

                        def V(fn, extra_r=()):
                            vcnt[0] += 1
                            if vcnt[0] > rstop:
                                return
                            S.op("dve", fn, reads=[brt, bconst] + list(extra_r), writes=[brt])
                        tid = t0 // 128 + i
                        if moe == "sorted":
                            oh1t = OH[:, tid * 64:tid * 64 + 32]
                            oh2t = OH[:, tid * 64 + 32:tid * 64 + 64]
                        else:
                            oh1t = T["oh1"][:, :]
                            oh2t = T["oh2"][:, :]
                        g3 = lambda t: t[:, 0:32].rearrange("p (g j) -> p g j", g=4)
                        g3a = lambda a: a.rearrange("p (g j) -> p g j", g=4)
                        bc3 = lambda t: t[:, 0:4].unsqueeze(2).to_broadcast([128, 4, 8])
                        V(lambda e: e.tensor_copy(out=T["lg"][:, :], in_=pl[:, 0:32]), [bpl])
                        V(lambda e: e.tensor_reduce(out=sc[:, 0:1], in_=T["lg"][:, :], axis=AX.X, op=ALU.max))
                        V(lambda e: e.tensor_scalar(out=sc[:, 1:2], in0=sc[:, 0:1], scalar1=-1.0, scalar2=None, op0=ALU.mult))
                        if rstop >= 4:
                            S.op("act", lambda e: e.activation(out=T["ex"][:, :], in_=T["lg"][:, :], func=AF.Exp, bias=sc[:, 1:2], scale=1.0),
                                 reads=[brt], writes=[brt])
                        V(lambda e: e.tensor_reduce(out=sc[:, 2:3], in_=T["ex"][:, :], axis=AX.X, op=ALU.add))
                        V(lambda e: e.reciprocal(out=sc[:, 3:4], in_=sc[:, 2:3]))
                        V(lambda e: e.tensor_scalar(out=T["pr"][:, :], in0=T["ex"][:, :], scalar1=sc[:, 3:4], scalar2=None, op0=ALU.mult))
                        V(lambda e: e.tensor_tensor(out=T["sel"][:, :], in0=T["pr"][:, :], in1=rbt[:, :], op=ALU.add))
                        V(lambda e: e.tensor_reduce(out=T["m1g"][:, :], in_=g3(T["sel"]), axis=AX.X, op=ALU.max))
                        V(lambda e: e.tensor_tensor(out=g3(T["eq"]), in0=g3(T["sel"]), in1=bc3(T["m1g"]), op=ALU.is_equal))
                        V(lambda e: e.scalar_tensor_tensor(out=T["sel2"][:, :], in0=T["eq"][:, :], scalar=-1e9, in1=T["sel"][:, :], op0=ALU.mult, op1=ALU.add))
                        V(lambda e: e.tensor_reduce(out=T["m2g"][:, :], in_=g3(T["sel2"]), axis=AX.X, op=ALU.max))
                        V(lambda e: e.tensor_tensor(out=T["gs"][:, :], in0=T["m1g"][:, :], in1=T["m2g"][:, :], op=ALU.add))
                        V(lambda e: e.tensor_reduce(out=sc[:, 4:5], in_=T["gs"][:, :], axis=AX.X, op=ALU.max))
                        V(lambda e: e.tensor_tensor(out=T["gh"][:, :], in0=T["gs"][:, :], in1=sc[:, 4:5].to_broadcast([128, 4]), op=ALU.is_equal))
                        V(lambda e: e.tensor_tensor(out=T["t4"][:, :], in0=T["gh"][:, :], in1=T["m1g"][:, :], op=ALU.mult))
                        V(lambda e: e.tensor_reduce(out=sc[:, 5:6], in_=T["t4"][:, :], axis=AX.X, op=ALU.add))
                        V(lambda e: e.tensor_tensor(out=T["t4"][:, :], in0=T["gh"][:, :], in1=T["m2g"][:, :], op=ALU.mult))
                        V(lambda e: e.tensor_reduce(out=sc[:, 6:7], in_=T["t4"][:, :], axis=AX.X, op=ALU.add))
                        V(lambda e, oh1t=oh1t, oh2t=oh2t: e.tensor_tensor(out=oh1t, in0=T["sel"][:, :], in1=sc[:, 5:6].to_broadcast([128, 32]), op=ALU.is_equal))
                        V(lambda e, oh1t=oh1t, oh2t=oh2t: e.tensor_tensor(out=g3a(oh1t), in0=g3a(oh1t), in1=bc3(T["gh"]), op=ALU.mult))
                        V(lambda e, oh1t=oh1t, oh2t=oh2t: e.tensor_tensor(out=oh2t, in0=T["sel"][:, :], in1=sc[:, 6:7].to_broadcast([128, 32]), op=ALU.is_equal))
                        V(lambda e, oh1t=oh1t, oh2t=oh2t: e.tensor_tensor(out=g3a(oh2t), in0=g3a(oh2t), in1=bc3(T["gh"]), op=ALU.mult))
                        V(lambda e, oh1t=oh1t, oh2t=oh2t: e.tensor_tensor(out=T["tmp"][:, :], in0=oh1t, in1=T["pr"][:, :], op=ALU.mult))
                        V(lambda e: e.tensor_reduce(out=sc[:, 7:8], in_=T["tmp"][:, :], axis=AX.X, op=ALU.add))
                        V(lambda e, oh1t=oh1t, oh2t=oh2t: e.tensor_tensor(out=T["tmp"][:, :], in0=oh2t, in1=T["pr"][:, :], op=ALU.mult))
                        V(lambda e: e.tensor_reduce(out=sc[:, 8:9], in_=T["tmp"][:, :], axis=AX.X, op=ALU.add))
                        V(lambda e: e.tensor_tensor(out=sc[:, 9:10], in0=sc[:, 7:8], in1=sc[:, 8:9], op=ALU.add))
                        V(lambda e: e.reciprocal(out=sc[:, 10:11], in_=sc[:, 9:10]))
                        V(lambda e: e.tensor_tensor(out=sc[:, 11:12], in0=sc[:, 7:8], in1=sc[:, 10:11], op=ALU.mult))
                        V(lambda e: e.tensor_tensor(out=sc[:, 12:13], in0=sc[:, 8:9], in1=sc[:, 10:11], op=ALU.mult))
                        if moe == "sorted":
                            S.op("dve", lambda e, tid=tid: e.tensor_copy(out=WW[:, 2 * tid:2 * tid + 2], in_=sc[:, 11:13]), reads=[brt], writes=[brt, brout])
                            continue
                        V(lambda e: e.tensor_scalar(out=T["tmp"][:, :], in0=T["oh1"][:, :], scalar1=sc[:, 11:12], scalar2=None, op0=ALU.mult))
                        if rstop < 10 ** 6:
                            continue
                        S.op("dve", lambda e: e.scalar_tensor_tensor(out=cwb[:, :], in0=T["oh2"][:, :], scalar=sc[:, 12:13], in1=T["tmp"][:, :], op0=ALU.mult, op1=ALU.add),
                             reads=[brt], writes=[bcwb])
                        ptp, bptp = psb[7], psB[7]
                        S.op("pe", lambda e: e.matmul(ptp[0:32, 0:128], cwb[:, :], identb[:, :], start=True, stop=True), reads=[bcwb, bconst], writes=[bptp])
                        tc0 = t0 + i * 128
                        S.op("act", lambda e, tc0=tc0: e.activation(out=cwT[:, tc0:tc0 + 128], in_=ptp[0:32, 0:128], func=AF.Copy), reads=[bptp], writes=[bcwT])
                if "cwTd" in taps:
                    S.dma("sp", lambda e: e.dma_start(out=cwTd[:, :], in_=cwT[:, :]), reads=[bcwT])
                S.barrier()
                S.emit()

        def phase_moe(l):
            _, groups = layer_groups(l)
            with ExitStack() as ph:
                h2r = Ring([(sb(ph, f"h2q{i}", [128, 16 * 512], BF16), Buf()) for i in range(1)])
                far = Ring([(sb(ph, f"fa{i}", [128, 16 * 512], F32), Buf()) for i in range(1)])
                wgr = Ring([(sb(ph, f"wg{i}", [128, 16 * 512], BF16), Buf()) for i in range(2)])
                wur = Ring([(sb(ph, f"wu{i}", [128, 16 * 512], BF16), Buf()) for i in range(2)])
                wdr = Ring([(sb(ph, f"wd{i}", [128, 4 * 2048], BF16), Buf()) for i in range(2)])
                cwr = Ring([(sb(ph, f"cws{i}", [128, 512], F32), Buf()) for i in range(2)])
                sgr = Ring([(sb(ph, f"sg{i}", [128, 512], F32), Buf()) for i in range(3)])
                tr = Ring([(sb(ph, f"tt{i}", [128, 512], F32), Buf()) for i in range(3)])
                acr = Ring([(sb(ph, f"ac{i}", [128, 4 * 512], BF16), Buf()) for i in range(2)])
                psg = Ring([(psb[i], psB[i]) for i in (0, 1)])
                psu = Ring([(psb[i], psB[i]) for i in (2, 3)])
                psy = Ring([(psb[i], psB[i]) for i in (4, 5, 6)])
                pbc, bpbc = psb[7], psB[7]
                for (t0, n, isc) in groups:
                    h2q, bh2 = h2r.next()
                    fa, bfa = far.next()
                    S.dma("sp", lambda e, h2q=h2q, t0=t0, n=n: e.dma_start(out=h2q[:, 0:16 * n].rearrange("p (k t) -> p k t", k=16),
                                                                          in_=h2T[:, :, t0:t0 + n].rearrange("k p t -> p k t")), writes=[bh2])
                    for ex in range(NE):
                        cws, bcws = cwr.next()
                        S.op("pe", lambda e, ex=ex, t0=t0, n=n: e.matmul(pbc[:, 0:n], selEb[0:32, ex * 128:(ex + 1) * 128], cwT[0:32, t0:t0 + n], start=True, stop=True),
                             reads=[bcwT, bconst], writes=[bpbc])
                        S.op("act", lambda e, cws=cws, n=n: e.activation(out=cws[:, 0:n], in_=pbc[:, 0:n], func=AF.Copy), reads=[bpbc], writes=[bcws])
                        for half in range(2):
                            wg, bwg = wgr.next()
                            wu, bwu = wur.next()
                            wd, bwd = wdr.next()
                            f0 = half * 512
                            S.dma("pool", lambda e, wg=wg, ex=ex, f0=f0: e.dma_start(out=wg[:, :].rearrange("p (k f) -> p k f", k=16),
                                                                                  in_=w_g[l, ex, :, f0:f0 + 512].rearrange("(k p) f -> p k f", p=128)), writes=[bwg])
                            S.dma("pool", lambda e, wu=wu, ex=ex, f0=f0: e.dma_start(out=wu[:, :].rearrange("p (k f) -> p k f", k=16),
                                                                                  in_=w_u[l, ex, :, f0:f0 + 512].rearrange("(k p) f -> p k f", p=128)), writes=[bwu])
                            S.dma("pool", lambda e, wd=wd, ex=ex, f0=f0: e.dma_start(out=wd[:, :].rearrange("p (j c) -> p j c", j=4),
                                                                                  in_=w_d[l, ex, f0:f0 + 512, :].rearrange("(j p) c -> p j c", p=128)), writes=[bwd])
                            ac, bac = acr.next()
                            for jf in range(4):
                                pg, bpg = psg.next()
                                pu, bpu = psu.next()
                                for k in range(16):
                                    S.op("pe", lambda e, pg=pg, wg=wg, k=k, jf=jf, h2q=h2q, n=n: e.matmul(
                                        pg[:, 0:n], wg[:, k * 512 + jf * 128:k * 512 + (jf + 1) * 128], h2q[:, k * n:(k + 1) * n],
                                        start=(k == 0), stop=(k == 15)), reads=[bwg, bh2], writes=[bpg], pe_chain=(k > 0))
                                for k in range(16):
                                    S.op("pe", lambda e, pu=pu, wu=wu, k=k, jf=jf, h2q=h2q, n=n: e.matmul(
                                        pu[:, 0:n], wu[:, k * 512 + jf * 128:k * 512 + (jf + 1) * 128], h2q[:, k * n:(k + 1) * n],
                                        start=(k == 0), stop=(k == 15)), reads=[bwu, bh2], writes=[bpu], pe_chain=(k > 0))
                                sg, bsg = sgr.next()
                                S.op("act", lambda e, sg=sg, pg=pg, n=n: e.activation(out=sg[:, 0:n], in_=pg[:, 0:n], func=AF.Silu), reads=[bpg], writes=[bsg])
                                tt, btt = tr.next()
                                S.op("dve", lambda e, tt=tt, sg=sg, pu=pu, n=n: e.tensor_tensor(out=tt[:, 0:n], in0=pu[:, 0:n], in1=sg[:, 0:n], op=ALU.mult),
                                     reads=[bpu, bsg], writes=[btt])
                                S.op("dve", lambda e, ac=ac, jf=jf, tt=tt, cws=cws, n=n: e.tensor_tensor(out=ac[:, jf * n:(jf + 1) * n], in0=tt[:, 0:n], in1=cws[:, 0:n], op=ALU.mult),
                                     reads=[btt, bcws], writes=[bac])
                            for c in range(16):
                                py, bpy = psy.next()
                                for jf in range(4):
                                    S.op("pe", lambda e, py=py, wd=wd, jf=jf, c=c, ac=ac, n=n: e.matmul(
                                        py[:, 0:n], wd[:, jf * 2048 + c * 128:jf * 2048 + (c + 1) * 128], ac[:, jf * n:(jf + 1) * n],
                                        start=(jf == 0), stop=(jf == 3)), reads=[bwd, bac], writes=[bpy], pe_chain=(jf > 0))
                                fc = fa[:, c * n:(c + 1) * n]
                                if ex == 0 and half == 0:
                                    S.op("dve", lambda e, fc=fc, py=py, n=n: e.tensor_copy(out=fc, in_=py[:, 0:n]), reads=[bpy], writes=[bfa])
                                else:
                                    S.op("dve", lambda e, fc=fc, py=py, n=n: e.tensor_tensor(out=fc, in0=fc, in1=py[:, 0:n], op=ALU.add), reads=[bpy, bfa], writes=[bfa])
                    S.dma("sp", lambda e, fa=fa, t0=t0, n=n: e.dma_start(out=fT[:, :, t0:t0 + n].rearrange("k p t -> p k t"),
                                                                        in_=fa[:, 0:16 * n].rearrange("p (k t) -> p k t", k=16)), reads=[bfa])
                S.barrier()
                S.emit()


        def layer_tiles(l):
            _, groups = layer_groups(l)
            return [t0 // 128 + i for (t0, n, isc) in groups for i in range(n // 128)]

        def n_blocks(l):
            return (len(layer_tiles(l)) * 256) // 128 + NE

        def phase_slots(l):
            tiles = layer_tiles(l)
            NB = n_blocks(l)
            with ExitStack() as ph:
                cum = sb(ph, "cum", [128, 32], F32)
                cumb = sb(ph, "cumb", [128, 32], BF16)
                mf = sb(ph, "mf", [128, 32], F32)
                mb = sb(ph, "mb", [128, 32], BF16)
                cnt = sb(ph, "cnt", [128, 32], F32)
                big = sb(ph, "big", [128, 104 * 32], F32)
                nbk = sb(ph, "nbk", [128, 32], F32)
                pst = sb(ph, "pst", [128, 33], F32)
                bef = sb(ph, "bef", [128, 128], F32)
                chf = sb(ph, "chf", [128, 128], F32)
                dst = sb(ph, "dst", [128, 32], F32)
                tm = sb(ph, "tm", [128, 32], F32)
                sf = sb(ph, "sf", [128, 2], F32)
                b1 = Buf()
                pp, bpp = psb[0], psB[0]

                def V(fn, extra_r=(), extra_w=()):
                    S.op("dve", fn, reads=[b1, bconst, brout] + list(extra_r), writes=[b1] + list(extra_w))
                V(lambda e: e.memset(cum[:, :], 0.0))
                for tid in tiles:
                    o1 = OH[:, tid * 64:tid * 64 + 32]
                    o2 = OH[:, tid * 64 + 32:tid * 64 + 64]
                    V(lambda e, o1=o1, o2=o2: e.tensor_tensor(out=mf[:, :], in0=o1, in1=o2, op=ALU.add))
                    V(lambda e: e.tensor_copy(out=mb[:, :], in_=mf[:, :]))
                    V(lambda e: e.tensor_copy(out=cumb[:, :], in_=cum[:, :]))
                    S.op("pe", lambda e: e.matmul(pp[:, 0:32], ustr[:, :], mb[:, :], start=True, stop=False), reads=[b1, bconst], writes=[bpp])
                    S.op("pe", lambda e: e.matmul(pp[:, 0:32], onesb[:, :], cumb[:, :], start=False, stop=True), reads=[b1, bconst], writes=[bpp], pe_chain=True)
                    V(lambda e, tid=tid: e.tensor_copy(out=POS[:, tid * 32:(tid + 1) * 32], in_=pp[:, 0:32]), [bpp])
                    V(lambda e: e.tensor_tensor(out=cum[:, :], in0=cum[:, :], in1=mf[:, :], op=ALU.add))
                V(lambda e: e.tensor_copy(out=cumb[:, :], in_=cum[:, :]))
                S.op("pe", lambda e: e.matmul(pp[:, 0:32], onesb[:, :], cumb[:, :], start=True, stop=True), reads=[b1, bconst], writes=[bpp])
                V(lambda e: e.tensor_copy(out=cnt[:, :], in_=pp[:, 0:32]), [bpp])
                J = 80
                V(lambda e: e.tensor_tensor(out=big[:, 0:32 * J].rearrange("p (a j) -> p a j", a=32),
                                            in0=cnt[:, :].unsqueeze(2).to_broadcast([128, 32, J]),
                                            in1=thr[:, 0:J].unsqueeze(1).to_broadcast([128, 32, J]), op=ALU.is_gt))
                V(lambda e: e.tensor_reduce(out=nbk[:, :], in_=big[:, 0:32 * J].rearrange("p (a j) -> p a j", a=32), axis=AX.X, op=ALU.add))
                V(lambda e: e.tensor_scalar(out=nbk[:, :], in0=nbk[:, :], scalar1=128.0, scalar2=None, op0=ALU.mult))
                V(lambda e: e.memset(pst[:, 0:1], 0.0))
                for ex in range(32):
                    V(lambda e, ex=ex: e.tensor_tensor(out=pst[:, ex + 1:ex + 2], in0=pst[:, ex:ex + 1], in1=nbk[:, ex:ex + 1], op=ALU.add))
                V(lambda e: e.tensor_tensor(out=big[:, 0:NB * 32].rearrange("p (b a) -> p b a", a=32),
                                            in0=pst[:, 1:33].unsqueeze(1).to_broadcast([128, NB, 32]),
                                            in1=thr[:, 0:NB].unsqueeze(2).to_broadcast([128, NB, 32]), op=ALU.is_le))
                V(lambda e: e.tensor_reduce(out=bef[:, 0:NB], in_=big[:, 0:NB * 32].rearrange("p (b a) -> p b a", a=32), axis=AX.X, op=ALU.add))
                V(lambda e: e.tensor_scalar(out=bef[:, 0:NB], in0=bef[:, 0:NB], scalar1=31.0, scalar2=None, op0=ALU.min))
                V(lambda e: e.tensor_copy(out=BLKE[:, 0:NB], in_=bef[:, 0:NB]), extra_w=[bblk])
                V(lambda e: e.memset(chf[:, 0:1], 1.0))
                V(lambda e: e.tensor_tensor(out=chf[:, 1:NB], in0=bef[:, 1:NB], in1=bef[:, 0:NB - 1], op=ALU.not_equal))
                V(lambda e: e.tensor_copy(out=CHG[:, 0:NB], in_=chf[:, 0:NB]), extra_w=[bblk])
                idf = sb(ph, "idf", [128, 128], F32)
                pen = sb(ph, "pen", [128, 128], F32)
                V(lambda e: e.tensor_scalar(out=pen[:, 0:NB], in0=chf[:, 0:NB], scalar1=-1.0, scalar2=-4.0e6, op0=ALU.add, op1=ALU.mult))
                for half in range(2):
                    for j in range(4):
                        V(lambda e, half=half, j=j: e.tensor_scalar(out=idf[:, 0:NB], in0=bef[:, 0:NB], scalar1=1024.0,
                                                                    scalar2=float((l * NE * 256 + half * 128) * 4 + j), op0=ALU.mult, op1=ALU.add))
                        V(lambda e: e.scalar_tensor_tensor(out=idf[:, 0:NB], in0=thr[:, 128:129].to_broadcast([128, NB]), scalar=4.0, in1=idf[:, 0:NB], op0=ALU.mult, op1=ALU.add))
                        V(lambda e: e.tensor_tensor(out=idf[:, 0:NB], in0=idf[:, 0:NB], in1=pen[:, 0:NB], op=ALU.add))
                        V(lambda e, half=half, j=j: e.tensor_copy(out=IDXT[:, (half * 4 + j) * 128:(half * 4 + j) * 128 + NB], in_=idf[:, 0:NB]), extra_w=[bblk])
                for tid in tiles:
                    V(lambda e, tid=tid: e.tensor_tensor(out=dst[:, :], in0=POS[:, tid * 32:(tid + 1) * 32], in1=pst[:, 0:32], op=ALU.add))
                    for j in range(2):
                        oh = OH[:, tid * 64 + j * 32:tid * 64 + (j + 1) * 32]
                        V(lambda e, oh=oh: e.tensor_tensor(out=tm[:, :], in0=dst[:, :], in1=oh, op=ALU.mult))
                        V(lambda e, j=j: e.tensor_reduce(out=sf[:, j:j + 1], in_=tm[:, :], axis=AX.X, op=ALU.add))
                    V(lambda e, tid=tid: e.tensor_copy(out=SLOT[:, 2 * tid:2 * tid + 2], in_=sf[:, :]), extra_w=[bslot])
                if "slotd" in taps:
                    S.dma("sp", lambda e: e.dma_start(out=slotd[:, :], in_=SLOT[:, :]), reads=[bslot])
                    S.dma("sp", lambda e: e.dma_start(out=blked[:, 0:128], in_=BLKE[:, :]), reads=[bblk])
                    S.dma("sp", lambda e: e.dma_start(out=blked[:, 128:256], in_=IDXT[:, 128:256]), reads=[bblk])
                S.barrier()
                S.emit()

        def phase_dispatch(l):
            tiles = layer_tiles(l)
            with ExitStack() as ph:
                htr = Ring([(sb(ph, f"ht{i}", [128, 2048], BF16), Buf()) for i in range(3)])
                for tid in tiles:
                    ht, bht = htr.next()
                    S.dma("sp", lambda e, ht=ht, tid=tid: e.dma_start(out=ht[:, :], in_=h2tok[tid * 128:(tid + 1) * 128, :]), writes=[bht])
                    for j in range(2):
                        S.dma("pool", lambda e, ht=ht, tid=tid, j=j: e.indirect_dma_start(
                            out=xsd[:, :], out_offset=bass.IndirectOffsetOnAxis(ap=SLOT[:, 2 * tid + j:2 * tid + j + 1], axis=0),
                            in_=ht[:, :], in_offset=None), reads=[bht, bslot])
                S.barrier()
                S.emit()

        regs = {}

        def phase_experts(l):
            NB = n_blocks(l)
            with ExitStack() as ph:
                wg = [(sb(ph, f"wgh{h}", [128, 16 * 512], BF16), [Buf() for _ in range(4)]) for h in range(2)]
                wu = [(sb(ph, f"wuh{h}", [128, 16 * 512], BF16), [Buf() for _ in range(4)]) for h in range(2)]
                wd = [(sb(ph, f"wdh{h}", [128, 4 * 2048], BF16), [Buf() for _ in range(4)]) for h in range(2)]
                xbr = Ring([(sb(ph, f"xb{i}", [128, 2048], BF16), Buf()) for i in range(2)])
                xtr = Ring([(sb(ph, f"xt{i}", [128, 2048], BF16), Buf()) for i in range(2)])
                sgr = Ring([(sb(ph, f"sg{i}", [128, 512], F32), Buf()) for i in range(2)])
                acr = Ring([(sb(ph, f"ac{i}", [128, 512], BF16), Buf()) for i in range(2)])
                atr = Ring([(sb(ph, f"act{i}", [128, 512], BF16), Buf()) for i in range(2)])
                ybr = Ring([(sb(ph, f"yb{i}", [128, 2048], F32), Buf()) for i in range(2)])
                def wgather(b, wsrc, wt, half):
                    for j in range(4):
                        def fn(e, j=j):
                            if "bv" not in regs:
                                reg = e.register("r_bound").__enter__()
                                e.reg_mov(reg, NROWS * 4 - 1)
                                regs["bv"] = e.snap(reg)
                            return e.indirect_dma_start(
                                out=wt[0][:, j * 2048:(j + 1) * 2048], out_offset=None, in_=wsrc[:, :],
                                in_offset=bass.IndirectOffsetOnAxis(ap=IDXT[:, (half * 4 + j) * 128 + b:(half * 4 + j) * 128 + b + 1], axis=0),
                                bounds_check=regs["bv"], oob_is_err=False)
                        S.dma("pool", fn, reads=[bblk], writes=[wt[1][j]])
                def gathers(b, half):
                    wgather(b, w_g, wg[half], half)
                    wgather(b, w_u, wu[half], half)
                    wgather(b, w_d, wd[half], half)

                fronts = {}

                def front(b):
                    xb, bxb = xbr.next()
                    S.dma("sp", lambda e, xb=xb, b=b: e.dma_start(out=xb[:, :], in_=xsd[b * 128:(b + 1) * 128, :]), writes=[bxb])
                    for k in range(16):
                        S.op("pe", lambda e, k=k, xb=xb: e.transpose(psbf[k // 8][:, (k % 8) * 128:(k % 8 + 1) * 128], xb[:, k * 128:(k + 1) * 128], identb[:, :]),
                             reads=[bxb, bconst], writes=[psB[6 + k // 8]])
                    xt, bxt = xtr.next()
                    S.op("act", lambda e, xt=xt: e.activation(out=xt[:, 0:1024], in_=psbf[0][:, :], func=AF.Copy), reads=[psB[6]], writes=[bxt])
                    S.op("dve", lambda e, xt=xt: e.tensor_copy(out=xt[:, 1024:2048], in_=psbf[1][:, :]), reads=[psB[7]], writes=[bxt])
                    fronts[b] = (xt, bxt)

                gathers(0, 0)
                gathers(0, 1)
                front(0)
                for b in range(NB):
                    xt, bxt = fronts.pop(b)
                    for half in range(2):
                        for (wt, pi) in ((wg[half], 0), (wu[half], 1)):
                            for k in range(16):
                                S.op("pe", lambda e, wt=wt, pi=pi, k=k, xt=xt: e.matmul(psb[pi][:, 0:512], xt[:, k * 128:(k + 1) * 128], wt[0][:, k * 512:(k + 1) * 512],
                                                                                   start=(k == 0), stop=(k == 15)), reads=[bxt, wt[1][k // 4]], writes=[psB[pi]], pe_chain=(k > 0))
                        if half == 0 and b + 1 < NB:
                            front(b + 1)
                        sg, bsg = sgr.next()
                        S.op("act", lambda e, sg=sg: e.activation(out=sg[:, :], in_=psb[0][:, 0:512], func=AF.Silu), reads=[psB[0]], writes=[bsg])
                        ac, bac = acr.next()
                        S.op("dve", lambda e, ac=ac, sg=sg: e.tensor_tensor(out=ac[:, :], in0=psb[1][:, 0:512], in1=sg[:, :], op=ALU.mult), reads=[psB[1], bsg], writes=[bac])
                        for jf in range(4):
                            S.op("pe", lambda e, jf=jf, ac=ac: e.transpose(psbf[0][:, jf * 128:(jf + 1) * 128], ac[:, jf * 128:(jf + 1) * 128], identb[:, :]),
                                 reads=[bac, bconst], writes=[psB[6]])
                        at, bat = atr.next()
                        S.op("act", lambda e, at=at: e.activation(out=at[:, :], in_=psbf[0][:, 0:512], func=AF.Copy), reads=[psB[6]], writes=[bat])
                        for c in range(4):
                            for jf in range(4):
                                first = (half == 0 and jf == 0)
                                last = (half == 1 and jf == 3)
                                S.op("pe", lambda e, c=c, jf=jf, at=at, half=half, first=first, last=last: e.matmul(
                                    psb[2 + c][:, 0:512], at[:, jf * 128:(jf + 1) * 128], wd[half][0][:, jf * 2048 + c * 512:jf * 2048 + (c + 1) * 512],
                                    start=first, stop=last), reads=[bat, wd[half][1][jf]], writes=[psB[2 + c]], pe_chain=(not first))
                        if b + 1 < NB:
                            gathers(b + 1, half)
                    yb, byb = ybr.next()
                    for c in range(4):
                        if c % 2 == 0:
                            S.op("act", lambda e, yb=yb, c=c: e.activation(out=yb[:, c * 512:(c + 1) * 512], in_=psb[2 + c][:, 0:512], func=AF.Copy), reads=[psB[2 + c]], writes=[byb])
                        else:
                            S.op("dve", lambda e, yb=yb, c=c: e.tensor_copy(out=yb[:, c * 512:(c + 1) * 512], in_=psb[2 + c][:, 0:512]), reads=[psB[2 + c]], writes=[byb])
                    S.dma("sp", lambda e, yb=yb, b=b: e.dma_start(out=ysd[b * 128:(b + 1) * 128, :], in_=yb[:, :]), reads=[byb])
                S.barrier()
                S.emit()

        def phase_combine(l):
            tiles = layer_tiles(l)
            with ExitStack() as ph:
                ar = Ring([(sb(ph, f"ga{i}", [128, 2048], F32), Buf()) for i in range(2)])
                br_ = Ring([(sb(ph, f"gb{i}", [128, 2048], F32), Buf()) for i in range(2)])
                fbr = Ring([(sb(ph, f"fb{i}", [128, 2048], BF16), Buf()) for i in range(2)])
                ftr = Ring([(sb(ph, f"ft{i}", [128, 2048], F32), Buf()) for i in range(2)])
                for tid in tiles:
                    a, ba = ar.next()
                    bb, bbb = br_.next()
                    S.dma("pool", lambda e, a=a, tid=tid: e.indirect_dma_start(
                        out=a[:, :], out_offset=None, in_=ysd[:, :], in_offset=bass.IndirectOffsetOnAxis(ap=SLOT[:, 2 * tid:2 * tid + 1], axis=0)),
                        reads=[bslot], writes=[ba])
                    S.dma("pool", lambda e, bb=bb, tid=tid: e.indirect_dma_start(
                        out=bb[:, :], out_offset=None, in_=ysd[:, :], in_offset=bass.IndirectOffsetOnAxis(ap=SLOT[:, 2 * tid + 1:2 * tid + 2], axis=0)),
                        reads=[bslot], writes=[bbb])
                    S.op("act", lambda e, a=a, tid=tid: e.activation(out=a[:, :], in_=a[:, :], func=AF.Identity, scale=WW[:, 2 * tid:2 * tid + 1]), reads=[ba, brout], writes=[ba])
                    fb, bfb = fbr.next()
                    S.op("dve", lambda e, fb=fb, a=a, bb=bb, tid=tid: e.scalar_tensor_tensor(out=fb[:, :], in0=bb[:, :], scalar=WW[:, 2 * tid + 1:2 * tid + 2], in1=a[:, :],
                                                                                       op0=ALU.mult, op1=ALU.add), reads=[ba, bbb, brout], writes=[bfb])
                    for k in range(16):
                        S.op("pe", lambda e, k=k, fb=fb: e.transpose(psbf[k // 8][:, (k % 8) * 128:(k % 8 + 1) * 128], fb[:, k * 128:(k + 1) * 128], identb[:, :]),
                             reads=[bfb, bconst], writes=[psB[6 + k // 8]])
                    ft, bft = ftr.next()
                    S.op("act", lambda e, ft=ft: e.activation(out=ft[:, 0:1024], in_=psbf[0][:, :], func=AF.Copy), reads=[psB[6]], writes=[bft])
                    S.op("dve", lambda e, ft=ft: e.tensor_copy(out=ft[:, 1024:2048], in_=psbf[1][:, :]), reads=[psB[7]], writes=[bft])
                    S.dma("sp", lambda e, ft=ft, tid=tid: e.dma_start(out=fT[:, :, tid * 128:(tid + 1) * 128].rearrange("k p t -> p k t"),
                                                                     in_=ft[:, :].rearrange("p (k t) -> p k t", k=16)), reads=[bft])
                S.barrier()
                S.emit()

        def phase_ln2(l):
            _, groups = layer_groups(l)
            with ExitStack() as ph:
                lnt_t = ln_tiles(ph)
                fsr = Ring([(sb(ph, f"fs{i}", [128, 16 * 512], F32), Buf()) for i in range(2)])
                xsr = Ring([(sb(ph, f"xs{i}", [128, 16 * 512], F32), Buf()) for i in range(2)])
                for (t0, n, isc) in groups:
                    s = 1 if isc else 0
                    fs, bfs = fsr.next()
                    xs, bxs = xsr.next()
                    S.dma("sp", lambda e, fs=fs, t0=t0, n=n: e.dma_start(out=fs[:, 0:16 * n].rearrange("p (k t) -> p k t", k=16),
                                                                        in_=fT[:, :, t0:t0 + n].rearrange("k p t -> p k t")), writes=[bfs])
                    S.dma("sp", lambda e, xs=xs, t0=t0, n=n: e.dma_start(out=xs[:, 0:16 * n].rearrange("p (k t) -> p k t", k=16),
                                                                        in_=x1T[:, :, t0:t0 + n].rearrange("k p t -> p k t")), writes=[bxs])
                    for c in range(16):
                        xc = xs[:, c * n:(c + 1) * n]
                        S.op("act", lambda e, xc=xc: e.activation(out=xc, in_=xc, func=AF.Copy, scale=ALPHA), reads=[bxs], writes=[bxs])
                        S.op("dve", lambda e, xc=xc, fs=fs, c=c, n=n, s=s: e.scalar_tensor_tensor(
                            out=xc, in0=fs[:, c * n:(c + 1) * n], scalar=mcol(l, 5, c, s), in1=xc, op0=ALU.mult, op1=ALU.add), reads=[bfs, bxs, bmod[l]], writes=[bxs])
                    layer_norm_fm(lnt_t, xs, bxs, n,
                                  lambda c: lnt[:, l * 64 + 32 + c:l * 64 + 32 + c + 1], lambda c: lnt[:, l * 64 + 48 + c:l * 64 + 48 + c + 1])
                    if l == nlayers - 1 and nlayers == 2:
                        dst = yout[:, :, t0 - HALO:t0 - HALO + n]
                    else:
                        dst = x2T[:, :, t0:t0 + n]
                    S.dma("sp", lambda e, xs=xs, dst=dst, n=n: e.dma_start(out=dst.rearrange("k p t -> p k t"),
                                                                         in_=xs[:, 0:16 * n].rearrange("p (k t) -> p k t", k=16)), reads=[bxs])
                S.barrier()
                S.emit()

        S.emit()
        tmpst.close()
        step = 0
        for l in range(nlayers):
            phase_mod(l)
        for l in range(nlayers):
            xres = xin if l == 0 else x2T
            if moe == "dense":
                fns = (lambda: phase_inproj(l, xres), lambda: phase_attn(l), lambda: phase_conv(l),
                       lambda: phase_out(l, xres), lambda: phase_moe(l), lambda: phase_ln2(l))
            else:
                fns = (lambda: phase_inproj(l, xres), lambda: phase_attn(l), lambda: phase_conv(l),
                       lambda: phase_out(l, xres), lambda: phase_slots(l), lambda: phase_dispatch(l),
                       lambda: phase_experts(l), lambda: phase_combine(l), lambda: phase_ln2(l))
            for fn in fns:
                step += 1
                if step <= upto:
                    fn()
        S.barrier()
        S.emit()
    return nc


def _consts():
    ident = np.eye(128, dtype=np.float32)
    m = np.arange(128)
    partner = np.where((m % 64) < 32, m + 32, m - 32)
    pswap = np.zeros((128, 128), np.float32)
    pswap[partner, m] = 1.0
    kk = np.arange(128)[:, None]
    qq = np.arange(128)[None, :]
    triL = np.tile((kk >= qq).astype(np.float32), (1, 4))
    triU = np.tile((kk <= qq).astype(np.float32), (1, 4))
    sel = np.zeros((128, 32 * 128), np.float32)
    for e in range(32):
        sel[e, e * 128:(e + 1) * 128] = 1.0
    return np.concatenate([ident, pswap, triL, triU, sel], axis=1)


def _fm(a):
    return np.ascontiguousarray(a.T.reshape(KC, 128, a.shape[0]))


def _pk(v):
    return np.ascontiguousarray(v.reshape(KC, 128).T)


def relayout_experts(w_gate, w_up, w_down):
    def gu(w):
        return np.ascontiguousarray(w.reshape(2, NE, 16, 128, 2, 512).transpose(0, 1, 4, 3, 2, 5)).reshape(2 * NE * 2 * 128 * 4, 2048)
    wd = np.ascontiguousarray(w_down.reshape(2, NE, 2, 4, 128, D).transpose(0, 1, 2, 4, 3, 5)).reshape(2 * NE * 2 * 128 * 4, 2048)
    return dict(w_gate=gu(w_gate), w_up=gu(w_up), w_down=wd)


def prep_core(core, x, c, ctx, c_ctx, b_ada, attn_sink, conv_w, ln1_g, ln1_b, ln2_g, ln2_b, router_w, router_b):
    b = core // 4
    s = (core % 4) * OWN
    pos = np.arange(s - HALO, s + OWN + HALO)
    valid = (pos >= 0) & (pos < 16384)
    xl = np.zeros((TL, D), np.float32)
    xl[valid] = x[b, pos[valid]]
    xin = np.concatenate([_fm(xl), _fm(ctx[b])], axis=2)
    cvec = np.stack([_pk(c[b]), _pk(c_ctx)], axis=2).reshape(128, 32)
    badaT = np.concatenate([np.ascontiguousarray(b_ada[l].reshape(96, 128).T) for l in range(2)], axis=1)
    lnp = np.concatenate([_pk(v[l]) for l in range(2) for v in (ln1_g, ln1_b, ln2_g, ln2_b)], axis=1)
    cwt = np.concatenate([np.stack([_pk(conv_w[l, j]) for j in range(3)], axis=2).reshape(128, 48) for l in range(2)], axis=1)
    sinkrow = np.repeat(attn_sink, 128, axis=1).astype(np.float32)
    rw = np.ascontiguousarray(router_w.reshape(KC, 128, NE).transpose(1, 0, 2)).reshape(128, 512)
    rb = np.tile(router_b[None, :], (128, 1)).astype(np.float32)
    posc = np.clip(pos, 0, 16383)
    row = (posc // 64).astype(np.float32)
    col = (posc % 64).astype(np.float32)
    inv = np.power(np.float32(10000.0), -np.arange(32, dtype=np.float32) / np.float32(32)).astype(np.float32)
    m = np.arange(128)
    axis = m // 64
    f = m % 32
    sign = np.where((m % 64) < 32, -1.0, 1.0).astype(np.float32)
    p_ax = np.where(axis[:, None] == 0, row[None, :], col[None, :]).astype(np.float32)
    ang = (p_ax * inv[f][:, None]).astype(np.float32)
    cosT = np.cos(ang).astype(np.float32)
    sinT = (np.sin(ang) * sign[:, None]).astype(np.float32)
    vb = np.where(valid, 0.0, -30000.0).astype(np.float32).reshape(36, 128).T
    vrow = np.tile(valid.astype(np.float32)[None, :], (128, 1))
    return dict(xin=xin, cvec=np.ascontiguousarray(cvec), badaT=np.ascontiguousarray(badaT), lnp=np.ascontiguousarray(lnp),
                cwt=np.ascontiguousarray(cwt), sinkrow=sinkrow, rw=rw, rb=rb, cosT=cosT, sinT=sinT,
                vbias=np.ascontiguousarray(vb), vrow=np.ascontiguousarray(vrow), consts=_consts(),
                thr=np.concatenate([np.tile((np.arange(128, dtype=np.float32) * 128.0)[None, :], (128, 1)), np.arange(128, dtype=np.float32)[:, None]], axis=1))


def kernel(x, c, ctx, c_ctx, w_ada, b_ada, w_in, attn_sink, conv_w, w_attn_proj, w_conv_proj, w_out,
           ln1_g, ln1_b, ln2_g, ln2_b, router_w, router_b, w_gate, w_up, w_down):
    g = lambda a: np.asarray(a, dtype=np.float32)
    x, c, ctx, c_ctx, b_ada, attn_sink, conv_w = g(x), g(c), g(ctx), g(c_ctx), g(b_ada), g(attn_sink), g(conv_w)
    ln1_g, ln1_b, ln2_g, ln2_b, router_w, router_b = g(ln1_g), g(ln1_b), g(ln2_g), g(ln2_b), g(router_w), g(router_b)
    shared = dict(w_ada=g(w_ada), w_in=g(w_in), w_attn_proj=g(w_attn_proj), w_conv_proj=g(w_conv_proj), w_out=g(w_out))
    shared.update(relayout_experts(g(w_gate), g(w_up), g(w_down)))
    nc = build()
    in_maps = []
    for core in range(8):
        m = prep_core(core, x, c, ctx, c_ctx, b_ada, attn_sink, conv_w, ln1_g, ln1_b, ln2_g, ln2_b, router_w, router_b)
        m.update(shared)
        in_maps.append(m)
    res = run_bass_kernel_spmd(nc, in_maps, core_ids=list(range(8)))
    out = np.empty((2, 16384, D), np.float32)
    for core in range(8):
        y = res.results[core]["yout"]
        b = core // 4
        s = (core % 4) * OWN
        out[b, s:s + OWN, :] = y.reshape(D, OWN).T
    return out
```
